# Optimizing a Trainium2 kernel written in Bass

```python
import math
import jax, jax.numpy as jnp
from jax import lax
import numpy as np

D_MODEL = 1024
BATCH = 1
SEQ = 16384
DEPTH = 4

HEAD_DIM = 64
BLK = 128
NEG_INF = -1e30

SWA_WINDOW = 128
A_Q_HEADS = 4
A_KV_HEADS = 2
A_GROUP = A_Q_HEADS // A_KV_HEADS
A_COLS = (A_Q_HEADS + 2 * A_KV_HEADS) * HEAD_DIM

DILATED_PAIRS = ((128, 1), (512, 4), (2048, 16))
B_N_GROUPS = len(DILATED_PAIRS)
B_HEADS_PER_GROUP = 4
B_HEADS = B_N_GROUPS * B_HEADS_PER_GROUP
B_COLS = 3 * B_HEADS * HEAD_DIM
SEQ_ALIGN = BLK * max(d for _, d in DILATED_PAIRS)

C_HEADS = 8
C_NOPE = 64
C_ROPE = 32
C_V = 64
Q_LORA = 256
KV_LORA = 128
ROPE_THETA = 10000.0
C_COLS = Q_LORA + KV_LORA + C_ROPE

N_IN = A_COLS + B_COLS + C_COLS
D_MIX = A_Q_HEADS * HEAD_DIM + B_HEADS_PER_GROUP * HEAD_DIM + C_HEADS * C_V

N_BUCKETS = 32
MAX_DISTANCE = 2048
N_BIAS_HEADS = A_Q_HEADS + B_HEADS

N_GROUPS = 4
EXPERTS_PER_GROUP = 8
N_EXPERTS = N_GROUPS * EXPERTS_PER_GROUP
TOP_K = 2
D_EXPERT = 256
ROW_BLK = 256

DEEPNORM_ALPHA = (2 * DEPTH) ** 0.25
DEEPNORM_BETA = (8 * DEPTH) ** -0.25

kernel_name = "hymba_swa_dilated_mla_hmoe_deepnorm"


def layer_norm(x, g, b, eps=1e-5):
    xf = x.astype(jnp.float32)
    mu = xf.mean(-1, keepdims=True)
    var = jnp.square(xf - mu).mean(-1, keepdims=True)
    return ((xf - mu) * lax.rsqrt(var + eps) * g + b).astype(x.dtype)


def rms_norm(x, g, eps=1e-6):
    xf = x.astype(jnp.float32)
    return (xf * lax.rsqrt(jnp.mean(xf * xf, -1, keepdims=True) + eps) * g).astype(x.dtype)


def rotary(x, cos, sin):
    x1, x2 = jnp.split(x, 2, axis=-1)
    return jnp.concatenate([x1 * cos - x2 * sin, x1 * sin + x2 * cos], -1).astype(x.dtype)


def t5_bucket(dist):
    max_exact = N_BUCKETS // 2
    large = max_exact + (jnp.log(jnp.maximum(dist, 1).astype(jnp.float32) / max_exact)
                         / math.log(MAX_DISTANCE / max_exact) * (N_BUCKETS - max_exact)).astype(jnp.int32)
    return jnp.where(dist < max_exact, dist, jnp.minimum(large, N_BUCKETS - 1))


def head_bias(table_cols, dist):
    return table_cols[t5_bucket(jnp.maximum(dist, 0))].transpose(2, 0, 1).astype(jnp.float32)


def band_blocks(t, n_blocks):
    tb = t.reshape((t.shape[0], n_blocks, BLK) + t.shape[2:])
    prev = jnp.pad(tb[:, :-1], ((0, 0), (1, 0)) + ((0, 0),) * (tb.ndim - 2))
    return jnp.concatenate([prev, tb], axis=2)


def banded_logits(q, k, bias, max_dist):
    Bp, L = q.shape[:2]
    n = L // BLK
    qb = q.reshape((Bp, n, BLK) + q.shape[2:])
    kb = band_blocks(k, n)
    s = jnp.einsum('bnqhgd,bnkhd->bnhgqk', qb, kb).astype(jnp.float32) * (q.shape[-1] ** -0.5) + bias
    qi = jnp.arange(BLK)[:, None]
    kj = jnp.arange(2 * BLK)[None, :]
    dist = BLK + qi - kj
    kpos = jnp.arange(n)[:, None, None] * BLK + kj[None] - BLK
    mask = (dist >= 0) & (dist <= max_dist) & (kpos >= 0)
    return jnp.where(mask[None, :, None, None], s, NEG_INF)


def swa_sink_mixer(h_a, sink, bias_a):
    Bsz, T, _ = h_a.shape
    q, k, v = jnp.split(h_a, [A_Q_HEADS * HEAD_DIM, (A_Q_HEADS + A_KV_HEADS) * HEAD_DIM], axis=-1)
    q = q.reshape(Bsz, T, A_KV_HEADS, A_GROUP, HEAD_DIM)
    k = k.reshape(Bsz, T, A_KV_HEADS, HEAD_DIM)
    v = v.reshape(Bsz, T, A_KV_HEADS, HEAD_DIM)
    s = banded_logits(q, k, bias_a, SWA_WINDOW - 1)
    sink_b = sink.astype(jnp.float32).reshape(1, 1, A_KV_HEADS, A_GROUP, 1, 1)
    m = jnp.maximum(s.max(-1, keepdims=True), sink_b)
    e = jnp.exp(s - m)
    p = e / (e.sum(-1, keepdims=True) + jnp.exp(sink_b - m))
    o = jnp.einsum('bnhgqk,bnkhd->bnqhgd', p.astype(v.dtype), band_blocks(v, T // BLK))
    return o.reshape(Bsz, T, A_Q_HEADS * HEAD_DIM)


def dilated_group(q, k, v, bias, dil, steps):
    Bsz, Tp, H, hd = q.shape
    Ls = Tp // dil
    n = Ls // BLK

    def to_sub(t):
        return t.reshape(Bsz, Ls, dil, H, hd).transpose(0, 2, 1, 3, 4).reshape(Bsz * dil, Ls, H, hd)

    qs, ks, vs = to_sub(q), to_sub(k), to_sub(v)
    s = banded_logits(qs[:, :, :, None], ks, bias[:, None], steps)
    m = s.max(-1, keepdims=True)
    e = jnp.exp(s - m)
    l = e.sum(-1, keepdims=True)
    lse = (m + jnp.log(l))[..., 0]
    o = jnp.einsum('bnhgqk,bnkhd->bnqhgd', (e / l).astype(v.dtype), band_blocks(vs, n))
    o = o.reshape(Bsz, dil, Ls, H, hd).transpose(0, 2, 1, 3, 4).reshape(Bsz, Tp, H, hd)
    lse = lse.transpose(0, 1, 4, 2, 3).reshape(Bsz, dil, Ls, H).transpose(0, 2, 1, 3).reshape(Bsz, Tp, H)
    return o, lse


def dilated_mixer(h_b, bias_b):
    Bsz, T, _ = h_b.shape
    T_pad = -(-T // SEQ_ALIGN) * SEQ_ALIGN
    h_b = jnp.pad(h_b, ((0, 0), (0, T_pad - T), (0, 0)))
    shape5 = (Bsz, T_pad, B_N_GROUPS, B_HEADS_PER_GROUP, HEAD_DIM)
    q, k, v = [t.reshape(shape5) for t in jnp.split(h_b, 3, axis=-1)]
    outs, lses = [], []
    for g, (window, dil) in enumerate(DILATED_PAIRS):
        o_g, lse_g = dilated_group(q[:, :, g], k[:, :, g], v[:, :, g], bias_b[g], dil, window // dil)
        outs.append(o_g)
        lses.append(lse_g)
    w = jax.nn.softmax(jnp.stack(lses), axis=0)
    o = jnp.einsum('gbth,gbthd->bthd', w.astype(h_b.dtype), jnp.stack(outs))
    return o[:, :T].reshape(Bsz, T, B_HEADS_PER_GROUP * HEAD_DIM)


def mla_mixer(h_c, q_norm_g, kv_norm_g, w_uq, w_ukv, cos, sin):
    Bsz, T, _ = h_c.shape
    c_q, c_kv, k_rope = jnp.split(h_c, [Q_LORA, Q_LORA + KV_LORA], axis=-1)
    q = (rms_norm(c_q, q_norm_g) @ w_uq).reshape(Bsz, T, C_HEADS, C_NOPE + C_ROPE)
    kv = (rms_norm(c_kv, kv_norm_g) @ w_ukv).reshape(Bsz, T, C_HEADS, C_NOPE + C_V)
    q_nope, q_rope = jnp.split(q, [C_NOPE], axis=-1)
    k_nope, v = jnp.split(kv, [C_NOPE], axis=-1)
    q_rope = rotary(q_rope, cos[:, None], sin[:, None])
    k_rope = rotary(k_rope, cos, sin)
    n = T // BLK
    scale = (C_NOPE + C_ROPE) ** -0.5
    key_pos = jnp.arange(T)

    def to_blocks(t):
        return t.reshape((Bsz, n, BLK) + t.shape[2:]).swapaxes(0, 1)

    def one_block(args):
        qn, qr, blk = args
        s = (jnp.einsum('bqhd,bkhd->bhqk', qn, k_nope)
             + jnp.einsum('bqhd,bkd->bhqk', qr, k_rope)).astype(jnp.float32) * scale
        q_pos = blk * BLK + jnp.arange(BLK)
        s = jnp.where(key_pos[None, :] <= q_pos[:, None], s, NEG_INF)
        p = jax.nn.softmax(s, axis=-1)
        return jnp.einsum('bhqk,bkhd->bqhd', p.astype(v.dtype), v)

    o = lax.map(one_block, (to_blocks(q_nope), to_blocks(q_rope), jnp.arange(n)))
    return o.swapaxes(0, 1).reshape(Bsz, T, C_HEADS * C_V)


def hybrid_mixer(x, w_in, w_out, sink, bias_a, bias_b, q_norm_g, kv_norm_g, w_uq, w_ukv, cos, sin):
    h = x @ w_in
    h_a, h_b, h_c = jnp.split(h, [A_COLS, A_COLS + B_COLS], axis=-1)
    o_a = swa_sink_mixer(h_a, sink, bias_a)
    o_b = dilated_mixer(h_b, bias_b)
    o_c = mla_mixer(h_c, q_norm_g, kv_norm_g, w_uq, w_ukv, cos, sin)
    return jnp.concatenate([o_a, o_b, o_c], axis=-1) @ w_out


def hierarchical_moe(h, w_rg, b_rg, w_re, b_re, w_gate, w_up, w_down):
    Bsz, T, D = h.shape
    n_tok = Bsz * T
    x = h.reshape(n_tok, D)
    g_logits = (x @ w_rg).astype(jnp.float32) + b_rg
    g_prob = jax.nn.softmax(g_logits, axis=-1)
    grp = jnp.argmax(g_logits, axis=-1)
    p_grp = jnp.take_along_axis(g_prob, grp[:, None], axis=-1)
    e_all = jnp.einsum('nd,gde->nge', x, w_re).astype(jnp.float32) + b_re
    e_logits = jnp.take_along_axis(e_all, grp[:, None, None], axis=1)[:, 0]
    top_val, top_idx = lax.top_k(e_logits, TOP_K)
    gate = jax.nn.softmax(top_val, axis=-1) * p_grp
    expert_ids = grp[:, None] * EXPERTS_PER_GROUP + top_idx
    A = n_tok * TOP_K
    flat_e = expert_ids.reshape(A).astype(jnp.int32)
    flat_w = gate.reshape(A)
    flat_tok = jnp.arange(A, dtype=jnp.int32) // TOP_K
    order = jnp.argsort(flat_e)
    e_sorted = flat_e[order]
    counts = jnp.zeros((N_EXPERTS,), jnp.int32).at[flat_e].add(1)
    padded = ((counts + ROW_BLK - 1) // ROW_BLK) * ROW_BLK
    pad_end = jnp.cumsum(padded)
    pad_start = pad_end - padded
    start = jnp.cumsum(counts) - counts
    dest = pad_start[e_sorted] + (jnp.arange(A, dtype=jnp.int32) - start[e_sorted])
    n_blocks = A // ROW_BLK + N_EXPERTS
    P = n_blocks * ROW_BLK
    row_tok = jnp.full((P,), n_tok, jnp.int32).at[dest].set(flat_tok[order])
    row_w = jnp.zeros((P,), h.dtype).at[dest].set(flat_w[order].astype(h.dtype))
    block_expert = jnp.minimum(
        jnp.searchsorted(pad_end, jnp.arange(n_blocks) * ROW_BLK, side='right'), N_EXPERTS - 1)
    x_pad = jnp.concatenate([x, jnp.zeros((1, D), x.dtype)], axis=0)
    rows = x_pad[row_tok].reshape(n_blocks, ROW_BLK, D)

    def block_ffn(args):
        xb, e = args
        return (jax.nn.silu(xb @ w_gate[e]) * (xb @ w_up[e])) @ w_down[e]

    ys = lax.map(block_ffn, (rows, block_expert)).reshape(P, D) * row_w[:, None]
    out = jnp.zeros((n_tok + 1, D), ys.dtype).at[row_tok].add(ys)[:n_tok]
    return out.reshape(Bsz, T, D)


def setup_inputs(seed: int = 0) -> dict:
    key = jax.random.key(seed)
    ks = jax.random.split(key, 20)
    f32 = jnp.float32

    def nrm(k, shape, scale):
        return jax.random.normal(k, shape, f32) * scale

    return {
        "x": nrm(ks[0], (BATCH, SEQ, D_MODEL), 1.0),
        "w_in": nrm(ks[1], (DEPTH, D_MODEL, N_IN), D_MODEL ** -0.5),
        "w_out": nrm(ks[2], (DEPTH, D_MIX, D_MODEL), DEEPNORM_BETA * D_MIX ** -0.5),
        "sinks": nrm(ks[3], (DEPTH, A_Q_HEADS), 0.5),
        "rel_bias": nrm(ks[4], (N_BUCKETS, N_BIAS_HEADS), 0.1),
        "mla_q_norm": 1.0 + nrm(ks[5], (DEPTH, Q_LORA), 0.05),
        "mla_kv_norm": 1.0 + nrm(ks[6], (DEPTH, KV_LORA), 0.05),
        "w_uq": nrm(ks[7], (DEPTH, Q_LORA, C_HEADS * (C_NOPE + C_ROPE)), Q_LORA ** -0.5),
        "w_ukv": nrm(ks[8], (DEPTH, KV_LORA, C_HEADS * (C_NOPE + C_V)), KV_LORA ** -0.5),
        "ln1_g": 1.0 + nrm(ks[9], (DEPTH, D_MODEL), 0.05),
        "ln1_b": nrm(ks[10], (DEPTH, D_MODEL), 0.02),
        "w_route_group": nrm(ks[11], (DEPTH, D_MODEL, N_GROUPS), D_MODEL ** -0.5),
        "b_route_group": nrm(ks[12], (DEPTH, N_GROUPS), 0.01),
        "w_route_expert": nrm(ks[13], (DEPTH, N_GROUPS, D_MODEL, EXPERTS_PER_GROUP), D_MODEL ** -0.5),
        "b_route_expert": nrm(ks[14], (DEPTH, N_GROUPS, EXPERTS_PER_GROUP), 0.01),
        "w_expert_gate": nrm(ks[15], (DEPTH, N_EXPERTS, D_MODEL, D_EXPERT), D_MODEL ** -0.5),
        "w_expert_up": nrm(ks[16], (DEPTH, N_EXPERTS, D_MODEL, D_EXPERT), D_MODEL ** -0.5),
        "w_expert_down": nrm(ks[17], (DEPTH, N_EXPERTS, D_EXPERT, D_MODEL), DEEPNORM_BETA * D_EXPERT ** -0.5),
        "ln2_g": 1.0 + nrm(ks[18], (DEPTH, D_MODEL), 0.05),
        "ln2_b": nrm(ks[19], (DEPTH, D_MODEL), 0.02),
    }


def reference(x, w_in, w_out, sinks, rel_bias, mla_q_norm, mla_kv_norm, w_uq, w_ukv, ln1_g, ln1_b,
              w_route_group, b_route_group, w_route_expert, b_route_expert,
              w_expert_gate, w_expert_up, w_expert_down, ln2_g, ln2_b):
    T = x.shape[1]
    inv_freq = ROPE_THETA ** (-jnp.arange(0, C_ROPE, 2, dtype=jnp.float32) / C_ROPE)
    ang = jnp.arange(T, dtype=jnp.float32)[:, None] * inv_freq[None, :]
    cos, sin = jnp.cos(ang), jnp.sin(ang)
    dist = BLK + jnp.arange(BLK)[:, None] - jnp.arange(2 * BLK)[None, :]
    bias_a = head_bias(rel_bias[:, :A_Q_HEADS], dist).reshape(A_KV_HEADS, A_GROUP, BLK, 2 * BLK)
    bias_b = [head_bias(rel_bias[:, A_Q_HEADS + g * B_HEADS_PER_GROUP:A_Q_HEADS + (g + 1) * B_HEADS_PER_GROUP],
                        dist * dil)
              for g, (_, dil) in enumerate(DILATED_PAIRS)]
    for layer in range(DEPTH):
        y = hybrid_mixer(x, w_in[layer], w_out[layer], sinks[layer], bias_a, bias_b,
                         mla_q_norm[layer], mla_kv_norm[layer], w_uq[layer], w_ukv[layer], cos, sin)
        x = layer_norm(DEEPNORM_ALPHA * x + y, ln1_g[layer], ln1_b[layer])
        y = hierarchical_moe(x, w_route_group[layer], b_route_group[layer], w_route_expert[layer],
                             b_route_expert[layer], w_expert_gate[layer], w_expert_up[layer],
                             w_expert_down[layer])
        x = layer_norm(DEEPNORM_ALPHA * x + y, ln2_g[layer], ln2_b[layer])
    return x
```

```python
import contextlib
import math
import numpy as np
import concourse.bass as bass
import concourse.mybir as mybir
from concourse.bass_utils import run_bass_kernel_spmd

F32 = mybir.dt.float32
BF16 = mybir.dt.bfloat16
I32 = mybir.dt.int32
AF = mybir.ActivationFunctionType
ALU = mybir.AluOpType
AX = mybir.AxisListType

EPOCH = 30000
COMPUTE = ("act", "dve", "pool", "pe")


class _Op:
    __slots__ = ("eng", "fn", "deps", "dma", "key", "seq", "val")


class Sched:
    def __init__(self, nc, stack):
        self.nc = nc
        self.stack = stack
        self.ops = []
        self.lastw = {}
        self.readers = {}
        self.nseq = {e: 0 for e in ("sync", "act", "dve", "pool", "pe")}
        self.dmacount = {}
        self.n_tensors = 0

    def sbuf(self, name, shape, dtype):
        return self.stack.enter_context(self.nc.sbuf_tensor(name, list(shape), dtype))

    def psum(self, name, shape, dtype):
        return self.stack.enter_context(self.nc.psum_tensor(name, list(shape), dtype))

    def add(self, eng, fn, reads=(), writes=(), dma=False, key=None):
        op = _Op()
        op.eng, op.fn, op.dma = eng, fn, dma
        idx = len(self.ops)
        deps = {}
        for r in reads:
            w = self.lastw.get(r)
            if w is not None:
                deps[w] = "raw"
        for w_ in writes:
            w = self.lastw.get(w_)
            if w is not None and w not in deps:
                deps[w] = "waw"
            for rd in self.readers.get(w_, ()):
                if rd not in deps:
                    deps[rd] = "war"
        op.deps = deps
        if dma:
            if key is None:
                r0 = writes[0]
                key = r0[0] if isinstance(r0, tuple) else r0
            op.key = key
            c = self.dmacount.get(key, 0) + 1
            self.dmacount[key] = c
            op.val = 16 * c
        else:
            op.key = None
            self.nseq[eng] += 1
            op.seq = self.nseq[eng]
        for w_ in writes:
            self.lastw[w_] = idx
            self.readers[w_] = []
        for r in reads:
            self.readers.setdefault(r, []).append(idx)
        self.ops.append(op)
        return idx

    def emit(self):
        nc, stack = self.nc, self.stack
        csem = {}
        for e in COMPUTE:
            n_ep = self.nseq[e] // EPOCH + 1
            csem[e] = [stack.enter_context(nc.semaphore(f"c_{e}_{i}")) for i in range(n_ep)]
        dsem = {k: stack.enter_context(nc.semaphore(f"d_{i}")) for i, k in enumerate(self.dmacount)}
        per_eng = {e: [] for e in self.nseq}
        waited = {e: {} for e in self.nseq}
        dmaseen = {k: 0 for k in self.dmacount}
        plan = []
        for op in self.ops:
            need = {}
            for d, kind in op.deps.items():
                dop = self.ops[d]
                if dop.dma:
                    s = dsem[dop.key]
                    v = dmaseen[dop.key]
                    assert v >= dop.val
                else:
                    if dop.eng == op.eng and not op.dma:
                        if op.eng == "pe" or kind != "raw":
                            continue
                    ep = (dop.seq - 1) // EPOCH
                    s = csem[dop.eng][ep]
                    v = dop.seq - ep * EPOCH
                if need.get(s, (0,))[0] < v:
                    need[s] = (v, s)
            waits = []
            wd = waited[op.eng]
            for s, (v, _) in need.items():
                if wd.get(s, 0) >= v:
                    continue
                wd[s] = v
                waits.append((s, v))
            if op.dma:
                dmaseen[op.key] = op.val
                inc = (dsem[op.key], 16)
            else:
                ep = (op.seq - 1) // EPOCH
                inc = (csem[op.eng][ep], 1)
            per_eng[op.eng].append((op.fn, waits, inc))
        self.per_eng = per_eng
        final_waits = [(dsem[k], 16 * c) for k, c in self.dmacount.items()]

        def run(engname, eng):
            for fn, waits, inc in per_eng[engname]:
                for s, v in waits:
                    eng.wait_ge(s, v)
                ins = fn(eng)
                ins.then_inc(inc[0], inc[1])

        with nc.Block() as block:
            @block.sync
            def _(e):
                run("sync", e)
                for s, v in final_waits:
                    e.wait_ge(s, v)

            @block.scalar
            def _(e):
                run("act", e)

            @block.vector
            def _(e):
                run("dve", e)

            @block.gpsimd
            def _(e):
                run("pool", e)

            @block.tensor
            def _(e):
                run("pe", e)


NC = 8
TL = 2048
NT = 16
D = 1024
N_IN = 3232
NEG = -30000.0


def _run(nc, in_maps):
    res = run_bass_kernel_spmd(nc, in_maps, core_ids=list(range(NC)))
    return res.results


def build_k1():
    nc = bass.Bass("TRN2", target_bir_lowering=False)
    xT = nc.dram_tensor("xT", [D, TL], F32, kind="ExternalInput").ap()
    w = nc.dram_tensor("w", [D, N_IN], F32, kind="ExternalInput").ap()
    h = nc.dram_tensor("h", [TL, N_IN], BF16, kind="ExternalOutput").ap()
    with contextlib.ExitStack() as stack:
        S = Sched(nc, stack)
        xf = [S.sbuf(f"xf{i}", [128, TL], F32) for i in range(2)]
        xb = S.sbuf("xb", [128, 8, TL], BF16)
        for kc in range(8):
            b = kc % 2
            S.add("sync", lambda e, kc=kc, b=b: e.dma_start(out=xf[b][:], in_=xT[kc * 128:(kc + 1) * 128, :]),
                  writes=[("xf", b)], dma=True, key=f"xf{b}")
            S.add("dve" if b else "pool", lambda e, kc=kc, b=b: e.tensor_copy(out=xb[:, kc, :], in_=xf[b][:]),
                  reads=[("xf", b)], writes=[("xb", kc)])
        wf = [S.sbuf(f"wf{i}", [128, 8, 512], F32) for i in range(2)]
        wb = [S.sbuf(f"wb{i}", [128, 8, 512], BF16) for i in range(2)]
        ps = [S.psum(f"ps{i}", [128, 512], F32) for i in range(4)]
        hs = [S.sbuf(f"hs{i}", [128, 512], BF16) for i in range(4)]
        wv = w.rearrange("(c p) n -> p c n", p=128)
        for cb in range(7):
            ncol = 512 if cb < 6 else N_IN - 6 * 512
            c0 = cb * 512
            b = cb % 2
            S.add("sync", lambda e, b=b, c0=c0, ncol=ncol: e.dma_start(out=wf[b][:, :, :ncol], in_=wv[:, :, c0:c0 + ncol]),
                  writes=[("wf", b)], dma=True, key=f"wf{b}")
            S.add("pool", lambda e, b=b, ncol=ncol: e.tensor_copy(out=wb[b][:, :, :ncol], in_=wf[b][:, :, :ncol]),
                  reads=[("wf", b)], writes=[("wb", b)])
            for tt in range(NT):
                i = (cb * NT + tt) % 4
                for kc in range(8):
                    S.add("pe", lambda e, i=i, kc=kc, tt=tt, b=b, ncol=ncol: e.matmul(
                        ps[i][:, :ncol], lhsT=xb[:, kc, tt * 128:(tt + 1) * 128], rhs=wb[b][:, kc, :ncol],
                        start=(kc == 0), stop=(kc == 7)),
                        reads=[("xb", kc), ("wb", b)], writes=[("ps", i)])
                S.add("act" if i % 2 else "dve", lambda e, i=i, ncol=ncol: (e.copy if i % 2 else e.tensor_copy)(
                    out=hs[i][:, :ncol], in_=ps[i][:, :ncol]), reads=[("ps", i)], writes=[("hs", i)])
                S.add("pool", lambda e, i=i, tt=tt, c0=c0, ncol=ncol: e.dma_start(
                    out=h[tt * 128:(tt + 1) * 128, c0:c0 + ncol], in_=hs[i][:, :ncol]),
                    reads=[("hs", i)], writes=[("h", cb, tt)], dma=True, key=f"hso{i}")
        S.emit()
    return nc


def build_k2():
    nc = bass.Bass("TRN2", target_bir_lowering=False)
    di = lambda n, s, t: nc.dram_tensor(n, s, t, kind="ExternalInput").ap()
    cqT = di("cqT", [256, TL], BF16)
    ckvT = di("ckvT", [128, TL], BF16)
    kr2 = di("kr2", [2, 32, TL], BF16)
    gq = di("gq", [128, 2], F32)
    gkv = di("gkv", [128, 1], F32)
    wuq = di("wuq", [256, 768], F32)
    wuqs = di("wuqs", [256, 768], F32)
    wk = di("wk", [128, 512], F32)
    wv = di("wv", [128, 512], F32)
    cs2 = di("cs2", [2, 32, TL], F32)
    qT = nc.dram_tensor("qT", [8, 96, TL], BF16, kind="ExternalOutput").ap()
    KT = nc.dram_tensor("KT", [8, 96, TL], BF16, kind="ExternalOutput").ap()
    V = nc.dram_tensor("V", [TL, 512], BF16, kind="ExternalOutput").ap()
    with contextlib.ExitStack() as stack:
        S = Sched(nc, stack)
        cq = S.sbuf("cq", [128, 2, TL], BF16)
        ckv = S.sbuf("ckv", [128, TL], BF16)
        kr = S.sbuf("kr", [96, TL], BF16)
        krs = S.sbuf("krs", [96, TL], BF16)
        C2 = S.sbuf("C2", [96, TL], F32)
        S2 = S.sbuf("S2", [96, TL], F32)
        gq_s = S.sbuf("gq_s", [128, 2], F32)
        gkv_s = S.sbuf("gkv_s", [128, 1], F32)
        wf = S.sbuf("wf", [128, 2, 768], F32)
        wuq_b = S.sbuf("wuq_b", [128, 2, 768], BF16)
        wuqs_b = S.sbuf("wuqs_b", [128, 2, 768], BF16)
        wkf = S.sbuf("wkf", [128, 512], F32)
        wk_b = S.sbuf("wk_b", [128, 512], BF16)
        wv_b = S.sbuf("wv_b", [128, 512], BF16)
        ones = S.sbuf("ones", [128, 128], F32)
        sq = S.sbuf("sq", [128, TL], F32)
        rq = S.sbuf("rq", [128, TL], F32)
        rk = S.sbuf("rk", [128, TL], F32)
        cqn = S.sbuf("cqn", [128, 2, TL], BF16)
        ckvn = S.sbuf("ckvn", [128, TL], BF16)
        krr = S.sbuf("krr", [96, TL], BF16)
        t1 = S.sbuf("t1", [96, TL], F32)
        t2 = S.sbuf("t2", [96, TL], F32)
        ld = lambda dst, src, res, key: S.add("sync", lambda e: e.dma_start(out=dst, in_=src), writes=[res], dma=True, key=key)
        ld(cq[:], cqT.rearrange("(c p) n -> p c n", p=128), "cq", "cq")
        ld(ckv[:], ckvT, "ckv", "ckv")
        ld(kr[64:96, :], kr2[0], "kr", "kr")
        ld(krs[64:96, :], kr2[1], "krs", "krs")
        ld(C2[64:96, :], cs2[0], "C2", "C2")
        ld(S2[64:96, :], cs2[1], "S2", "S2")
        ld(gq_s[:], gq, "gq", "gq")
        ld(gkv_s[:], gkv, "gkv", "gkv")
        S.add("pool", lambda e: e.memset(ones[:], 1.0), writes=["ones"])
        epsc = S.sbuf("epsc", [128, 1], F32)
        S.add("pool", lambda e: e.memset(epsc[:], 1e-6), writes=["epsc"])
        ld(wf[:], wuq.rearrange("(c p) n -> p c n", p=128), "wf", "wf")
        S.add("dve", lambda e: e.tensor_copy(out=wuq_b[:], in_=wf[:]), reads=["wf"], writes=["wuq_b"])
        ld(wf[:], wuqs.rearrange("(c p) n -> p c n", p=128), "wf", "wf")
        S.add("dve", lambda e: e.tensor_copy(out=wuqs_b[:], in_=wf[:]), reads=["wf"], writes=["wuqs_b"])
        ld(wkf[:], wk, "wkf", "wkf")
        S.add("dve", lambda e: e.tensor_copy(out=wk_b[:], in_=wkf[:]), reads=["wkf"], writes=["wk_b"])
        ld(wkf[:], wv, "wkf", "wkf")
        S.add("dve", lambda e: e.tensor_copy(out=wv_b[:], in_=wkf[:]), reads=["wkf"], writes=["wv_b"])
        ps = [S.psum(f"ps{i}", [128, 512], F32) for i in range(6)]
        pi = [0]

        def nps():
            pi[0] = (pi[0] + 1) % 6
            return pi[0]

        def rstd(src_chunks, nfeat, eps, out_r, tag):
            nchunk = len(src_chunks)
            for g4 in range(4):
                cs = slice(g4 * 512, (g4 + 1) * 512)
                i = nps()
                for kc, (src, res) in enumerate(src_chunks):
                    S.add("dve", lambda e, src=src, cs=cs: e.tensor_tensor(out=sq[:, cs], in0=src[:, cs], in1=src[:, cs], op=ALU.mult),
                          reads=[res], writes=[("sq", g4)])
                    S.add("pe", lambda e, i=i, cs=cs, kc=kc: e.matmul(ps[i][:], lhsT=ones[:], rhs=sq[:, cs], start=(kc == 0), stop=(kc == nchunk - 1)),
                          reads=["ones", ("sq", g4)], writes=[("ps", i)])
                S.add("act", lambda e, i=i, cs=cs: e.activation(out=out_r[:, cs], in_=ps[i][:], func=AF.Sqrt, bias=epsc[:, 0:1], scale=1.0 / nfeat),
                      reads=[("ps", i), "epsc"], writes=[(tag, g4)])
                S.add("dve", lambda e, cs=cs: e.reciprocal(out=out_r[:, cs], in_=out_r[:, cs]),
                      reads=[(tag, g4)], writes=[(tag, g4)])

        rstd([(cq[:, 0, :], "cq"), (cq[:, 1, :], "cq")], 256, 1e-6, rq, "rq")
        for kc in range(2):
            for g4 in range(4):
                cs = slice(g4 * 512, (g4 + 1) * 512)
                S.add("dve", lambda e, kc=kc, cs=cs: e.scalar_tensor_tensor(out=cqn[:, kc, cs], in0=cq[:, kc, cs], scalar=gq_s[:, kc:kc + 1], in1=rq[:, cs], op0=ALU.mult, op1=ALU.mult),
                      reads=["cq", "gq", ("rq", g4)], writes=[("cqn", kc, g4)])
        rstd([(ckv, "ckv")], 128, 1e-6, rk, "rk")
        for g4 in range(4):
            cs = slice(g4 * 512, (g4 + 1) * 512)
            S.add("dve", lambda e, cs=cs: e.scalar_tensor_tensor(out=ckvn[:, cs], in0=ckv[:, cs], scalar=gkv_s[:, 0:1], in1=rk[:, cs], op0=ALU.mult, op1=ALU.mult),
                  reads=["ckv", "gkv", ("rk", g4)], writes=[("ckvn", g4)])
        S.add("dve", lambda e: e.tensor_tensor(out=t1[64:96, :], in0=kr[64:96, :], in1=C2[64:96, :], op=ALU.mult), reads=["kr", "C2"], writes=["t1"])
        S.add("pool", lambda e: e.tensor_tensor(out=t2[64:96, :], in0=krs[64:96, :], in1=S2[64:96, :], op=ALU.mult), reads=["krs", "S2"], writes=["t2"])
        S.add("dve", lambda e: e.tensor_tensor(out=krr[64:96, :], in0=t1[64:96, :], in1=t2[64:96, :], op=ALU.add), reads=["t1", "t2"], writes=["krr"])
        for h in range(8):
            S.add("pool", lambda e, h=h: e.dma_start(out=KT[h, 64:96, :], in_=krr[64:96, :]), reads=["krr"], writes=[("KTr", h)], dma=True, key="krro")
        qs = [S.sbuf(f"qs{i}", [96, 512], BF16) for i in range(2)]
        ks = [S.sbuf(f"ks{i}", [64, 512], BF16) for i in range(2)]
        u1 = [S.sbuf(f"u1{i}", [96, 512], F32) for i in range(2)]
        u2 = [S.sbuf(f"u2{i}", [96, 512], F32) for i in range(2)]
        n = 0
        for h in range(8):
            for g4 in range(4):
                cs = slice(g4 * 512, (g4 + 1) * 512)
                b = n % 2
                n += 1
                ia, ib, ik = nps(), nps(), nps()
                for kc in range(2):
                    S.add("pe", lambda e, ia=ia, kc=kc, h=h, cs=cs: e.matmul(ps[ia][0:96, :], lhsT=wuq_b[:, kc, h * 96:(h + 1) * 96], rhs=cqn[:, kc, cs], start=(kc == 0), stop=(kc == 1)),
                          reads=["wuq_b", ("cqn", kc, g4)], writes=[("ps", ia)])
                for kc in range(2):
                    S.add("pe", lambda e, ib=ib, kc=kc, h=h, cs=cs: e.matmul(ps[ib][0:96, :], lhsT=wuqs_b[:, kc, h * 96:(h + 1) * 96], rhs=cqn[:, kc, cs], start=(kc == 0), stop=(kc == 1)),
                          reads=["wuqs_b", ("cqn", kc, g4)], writes=[("ps", ib)])
                S.add("pe", lambda e, ik=ik, h=h, cs=cs: e.matmul(ps[ik][0:64, :], lhsT=wk_b[:, h * 64:(h + 1) * 64], rhs=ckvn[:, cs], start=True, stop=True),
                      reads=["wk_b", ("ckvn", g4)], writes=[("ps", ik)])
                S.add("act", lambda e, ia=ia, b=b: e.copy(out=qs[b][0:64, :], in_=ps[ia][0:64, :]), reads=[("ps", ia)], writes=[("qs", b, 0)])
                S.add("dve", lambda e, ia=ia, b=b, cs=cs: e.tensor_tensor(out=u1[b][64:96, :], in0=ps[ia][64:96, :], in1=C2[64:96, cs], op=ALU.mult),
                      reads=[("ps", ia), "C2"], writes=[("u1", b)])
                S.add("dve", lambda e, ib=ib, b=b, cs=cs: e.tensor_tensor(out=u2[b][64:96, :], in0=ps[ib][64:96, :], in1=S2[64:96, cs], op=ALU.mult),
                      reads=[("ps", ib), "S2"], writes=[("u2", b)])
                S.add("pool", lambda e, b=b: e.tensor_tensor(out=qs[b][64:96, :], in0=u1[b][64:96, :], in1=u2[b][64:96, :], op=ALU.add),
                      reads=[("u1", b), ("u2", b)], writes=[("qs", b, 1)])
                S.add("act", lambda e, ik=ik, b=b: e.copy(out=ks[b][:], in_=ps[ik][0:64, :]), reads=[("ps", ik)], writes=[("ks", b)])
                S.add("sync", lambda e, b=b, h=h, cs=cs: e.dma_start(out=qT[h, :, cs], in_=qs[b][:]), reads=[("qs", b, 0), ("qs", b, 1)], writes=[("qT", h, g4)], dma=True, key=f"qso{b}")
                S.add("sync", lambda e, b=b, h=h, cs=cs: e.dma_start(out=KT[h, 0:64, cs], in_=ks[b][:]), reads=[("ks", b)], writes=[("KTn", h, g4)], dma=True, key=f"kso{b}")
        vs = [S.sbuf(f"vs{i}", [128, 512], BF16) for i in range(2)]
        for tt in range(NT):
            b = tt % 2
            i = nps()
            S.add("pe", lambda e, i=i, tt=tt: e.matmul(ps[i][:], lhsT=ckvn[:, tt * 128:(tt + 1) * 128], rhs=wv_b[:], start=True, stop=True),
                  reads=["wv_b", ("ckvn", tt // 4)], writes=[("ps", i)])
            S.add("act", lambda e, i=i, b=b: e.copy(out=vs[b][:], in_=ps[i][:]), reads=[("ps", i)], writes=[("vs", b)])
            S.add("sync", lambda e, b=b, tt=tt: e.dma_start(out=V[tt * 128:(tt + 1) * 128, :], in_=vs[b][:]), reads=[("vs", b)], writes=[("V", tt)], dma=True, key=f"vso{b}")
        S.emit()
    return nc


def _rope_tables(c):
    inv = (10000.0 ** (-np.arange(0, 32, 2, dtype=np.float32) / 32)).astype(np.float32)
    pos = np.arange(c * TL, (c + 1) * TL, dtype=np.float32)
    ang = (pos[:, None] * inv[None, :]).astype(np.float32)
    cos, sin = np.cos(ang).astype(np.float32).T, np.sin(ang).astype(np.float32).T
    return np.ascontiguousarray(np.stack([np.concatenate([cos, cos], 0), np.concatenate([-sin, sin], 0)], 0))


def k2_inputs(h, d, l):
    wuq = d["w_uq"][l]
    wuqs = np.zeros_like(wuq)
    for hh in range(8):
        b = hh * 96 + 64
        wuqs[:, b:b + 16] = wuq[:, b + 16:b + 32]
        wuqs[:, b + 16:b + 32] = wuq[:, b:b + 16]
    wukv = d["w_ukv"][l].reshape(128, 8, 128)
    wk = np.ascontiguousarray(wukv[:, :, :64].reshape(128, 512))
    wv = np.ascontiguousarray(wukv[:, :, 64:].reshape(128, 512))
    gq = np.ascontiguousarray(d["mla_q_norm"][l].reshape(2, 128).T)
    gkv = np.ascontiguousarray(d["mla_kv_norm"][l].reshape(128, 1))
    maps = []
    for c in range(NC):
        hc = h[c * TL:(c + 1) * TL]
        krT = hc[:, 3200:3232].T
        maps.append({
            "cqT": np.ascontiguousarray(hc[:, 2816:3072].T),
            "ckvT": np.ascontiguousarray(hc[:, 3072:3200].T),
            "kr2": np.ascontiguousarray(np.stack([krT, np.concatenate([krT[16:], krT[:16]], 0)], 0)),
            "gq": gq, "gkv": gkv, "wuq": wuq, "wuqs": wuqs, "wk": wk, "wv": wv,
            "cs2": _rope_tables(c),
        })
    return maps


SCALE_C = 96 ** -0.5
NSTREAM = 16


def _stream_dil(s):
    return 1 if s < 4 else (1, 4, 16)[(s - 4) // 4]


def build_k3():
    nc = bass.Bass("TRN2", target_bir_lowering=False)
    di = lambda n, s, t: nc.dram_tensor(n, s, t, kind="ExternalInput").ap()
    qT = di("qT", [8, 96, TL], BF16)
    KTp = di("KTp", [8, 96, 7 * TL], BF16)
    KTo = di("KTo", [8, 96, TL], BF16)
    Vp = di("Vp", [8, 128, 112, 64], BF16)
    Vo = di("Vo", [8, 128, 16, 64], BF16)
    vis = di("vis", [128, 7], F32)
    mk = di("mk", [128, 4, 512], F32)
    QTb = di("QTb", [NSTREAM, 64, TL], BF16)
    KTb = di("KTb", [NSTREAM, 64, 16, 256], BF16)
    Vb = di("Vb", [NSTREAM, 128, 32, 64], BF16)
    bT = di("bT", [128, NSTREAM, 2, 256], F32)
    oc = nc.dram_tensor("oc", [TL, 512], BF16, kind="ExternalOutput").ap()
    Oab = nc.dram_tensor("Oab", [NSTREAM, TL, 65], F32, kind="ExternalOutput").ap()
    with contextlib.ExitStack() as stack:
        S = Sched(nc, stack)
        vis_s = S.sbuf("vis_s", [128, 7], F32)
        mk_s = S.sbuf("mk_s", [128, 4, 512], F32)
        bT_s = S.sbuf("bT_s", [128, NSTREAM, 2, 256], F32)
        ld = lambda eng, dst, src, res, key: S.add(eng, lambda e: e.dma_start(out=dst, in_=src), writes=[res], dma=True, key=key)
        ld("sync", vis_s[:], vis, "vis", "vis")
        ld("sync", mk_s[:], mk, "mk", "mk")
        ld("sync", bT_s[:], bT, "bT", "bT")
        Sps = [S.psum(f"Sps{i}", [128, 512], F32) for i in range(4)]
        Ops = [S.psum(f"Ops{i}", [128, 512], F32) for i in range(2)]
        Bps = [S.psum(f"Bps{i}", [128, 4, 65], F32) for i in range(2)]
        Pt = [S.sbuf(f"Pt{i}", [128, 512], BF16) for i in range(4)]
        tmp = [S.sbuf(f"tmp{i}", [128, 512], F32) for i in range(2)]
        qb = [S.sbuf(f"qb{i}", [64, TL], BF16) for i in range(2)]
        kb = [S.sbuf(f"kb{i}", [64, 16, 256], BF16) for i in range(2)]
        vb = [S.sbuf(f"vb{i}", [128, 32, 65], BF16) for i in range(2)]
        obs = [S.sbuf(f"obs{i}", [128, 16, 65], F32) for i in range(2)]
        for i in range(2):
            S.add("pool", lambda e, i=i: e.memset(vb[i][:], 1.0), writes=[("vb", i)])
        LA = 2
        items = []
        cnt = 0
        for s in range(NSTREAM):
            sb_ = s % 2
            d = _stream_dil(s)
            per = 16 // d
            for pr in range(8):
                si = cnt % 4
                ti = cnt % 2
                bi = cnt % 2
                cnt += 1

                def front(s=s, sb_=sb_, per=per, pr=pr, si=si, ti=ti):
                    if pr == 0:
                        ld("sync", qb[sb_][:], QTb[s], ("qb", sb_), f"qb{sb_}")
                        ld("sync", kb[sb_][:], KTb[s], ("kb", sb_), f"kb{sb_}")
                        S.add("sync", lambda e: e.dma_start(out=vb[sb_][:, :, 0:64], in_=Vb[s]), reads=[("vb", sb_)], writes=[("vb", sb_)], dma=True, key=f"vb{sb_}")
                    for j in range(2):
                        b = pr * 2 + j
                        for half in range(2):
                            S.add("pe", lambda e, j=j, half=half, b=b: e.matmul(
                                Sps[si][:, j * 256 + half * 128: j * 256 + half * 128 + 128], lhsT=kb[sb_][:, b, half * 128:(half + 1) * 128],
                                rhs=qb[sb_][:, b * 128:(b + 1) * 128], start=True, stop=True),
                                reads=[("kb", sb_), ("qb", sb_)], writes=[("S", si)])
                    for j in range(2):
                        b = pr * 2 + j
                        var = 0 if (b % per) == 0 else 1
                        S.add("dve", lambda e, j=j, var=var: e.scalar_tensor_tensor(
                            out=tmp[ti][:, j * 256:(j + 1) * 256], in0=Sps[si][:, j * 256:(j + 1) * 256], scalar=0.125,
                            in1=bT_s[:, s, var, :], op0=ALU.mult, op1=ALU.add),
                            reads=[("S", si), "bT"], writes=[("tmp", ti, j)])
                    S.add("act", lambda e: e.activation(out=Pt[si][:], in_=tmp[ti][:], func=AF.Exp),
                          reads=[("tmp", ti, 0), ("tmp", ti, 1)], writes=[("P", si)])

                def back(s=s, sb_=sb_, pr=pr, si=si, bi=bi):
                    for j in range(2):
                        b = pr * 2 + j
                        for half in range(2):
                            S.add("pe", lambda e, j=j, half=half, b=b: e.matmul(
                                Bps[bi][:, j, :], lhsT=Pt[si][:, j * 256 + half * 128: j * 256 + half * 128 + 128],
                                rhs=vb[sb_][:, b * 2 + half, :], start=(half == 0), stop=(half == 1)),
                                reads=[("P", si), ("vb", sb_)], writes=[("Bps", bi)])
                    S.add("dve", lambda e: e.tensor_copy(out=obs[sb_][:, pr * 2:pr * 2 + 2, :], in_=Bps[bi][:, 0:2, :]),
                          reads=[("Bps", bi)], writes=[("obs", sb_)])
                    if pr == 7:
                        S.add("pool", lambda e: e.dma_start(out=Oab[s].rearrange("(b p) d -> p b d", p=128), in_=obs[sb_][:]),
                              reads=[("obs", sb_)], writes=[("Oab", s)], dma=True, key=f"obso{sb_}")

                items.append((front, back))
        kp = [S.sbuf(f"kp{i}", [96, 7 * TL], BF16) for i in range(1)] * 2
        ko = [S.sbuf(f"ko{i}", [96, TL], BF16) for i in range(2)]
        vp = [S.sbuf(f"vp{i}", [128, 112, 65], BF16) for i in range(1)] * 2
        vo = [S.sbuf(f"vo{i}", [128, 16, 65], BF16) for i in range(2)]
        qh = [S.sbuf(f"qh{i}", [96, TL], BF16) for i in range(2)]
        OsT = [S.sbuf(f"OsT{i}", [65, 512], F32) for i in range(2)]
        Osb = [S.sbuf(f"Osb{i}", [128, 4, 65], F32) for i in range(2)]
        rc = [S.sbuf(f"rc{i}", [128, 4, 1], F32) for i in range(2)]
        ocs = S.sbuf("ocs", [128, NT, 512], BF16)
        identf = S.sbuf("identf", [128, 128], F32)
        S.add("pool", lambda e: e.memset(identf[:], 1.0), writes=["identf"])
        S.add("pool", lambda e: e.affine_select(out=identf[:], in_=identf[:], pattern=[[-1, 128]], compare_op=ALU.is_equal, fill=0.0, base=0, channel_multiplier=1),
              reads=["identf"], writes=["identf"])
        S.add("pool", lambda e: e.memset(vp[0][:], 1.0), writes=[("vp", 0)])
        for i in range(2):
            S.add("pool", lambda e, i=i: e.memset(vo[i][:], 1.0), writes=[("vo", i)])
        ocnt = 0
        for h in range(8):
            hb = h % 2
            for qt in range(4):
                ob = ocnt % 2
                ocnt += 1
                tiles = [("r", r, t) for r in range(7) for t in range(16)] + [("l", kt, 0) for kt in range(4 * qt + 4)]
                last = len(tiles) - 1
                qs_ = slice(qt * 512, (qt + 1) * 512)
                for idx, (kind, a, t) in enumerate(tiles):
                    si = cnt % 4
                    ti = cnt % 2
                    cnt += 1
                    if kind == "r":
                        lhsT = kp[hb][:, (a * 16 + t) * 128:(a * 16 + t + 1) * 128]
                        vt = vp[hb][:, a * 16 + t, :]
                        kres, vres = ("kp", 0), ("vp", 0)
                    else:
                        lhsT = ko[hb][:, a * 128:(a + 1) * 128]
                        vt = vo[hb][:, a, :]
                        kres, vres = ("ko", hb), ("vo", hb)

                    def front(h=h, hb=hb, qt=qt, idx=idx, kind=kind, a=a, si=si, ti=ti, lhsT=lhsT, kres=kres, qs_=qs_):
                        if qt == 0 and idx == 0:
                            ld("sync", qh[hb][:], qT[h], ("qh", hb), f"qh{hb}")
                            ld("sync", ko[hb][:], KTo[h], ("ko", hb), f"ko{hb}")
                            S.add("sync", lambda e: e.dma_start(out=vo[hb][:, :, 0:64], in_=Vo[h]), reads=[("vo", hb)], writes=[("vo", hb)], dma=True, key=f"vo{hb}")
                            ld("sync", kp[0][:], KTp[h], ("kp", 0), "kp0")
                            S.add("sync", lambda e: e.dma_start(out=vp[0][:, :, 0:64], in_=Vp[h]), reads=[("vp", 0)], writes=[("vp", 0)], dma=True, key="vp0")
                        S.add("pe", lambda e: e.matmul(Sps[si][:], lhsT=lhsT, rhs=qh[hb][:, qs_], start=True, stop=True),
                              reads=[kres, ("qh", hb)], writes=[("S", si)])
                        if kind == "r":
                            S.add("act", lambda e: e.activation(out=Pt[si][:], in_=Sps[si][:], func=AF.Exp, bias=vis_s[:, a:a + 1], scale=SCALE_C),
                                  reads=[("S", si), "vis"], writes=[("P", si)])
                        elif a < 4 * qt:
                            S.add("act", lambda e: e.activation(out=Pt[si][:], in_=Sps[si][:], func=AF.Exp, scale=SCALE_C),
                                  reads=[("S", si)], writes=[("P", si)])
                        else:
                            m = a - 4 * qt
                            S.add("dve", lambda e: e.scalar_tensor_tensor(out=tmp[ti][:], in0=Sps[si][:], scalar=SCALE_C, in1=mk_s[:, m, :], op0=ALU.mult, op1=ALU.add),
                                  reads=[("S", si), "mk"], writes=[("tmp", ti, 0), ("tmp", ti, 1)])
                            S.add("act", lambda e: e.activation(out=Pt[si][:], in_=tmp[ti][:], func=AF.Exp),
                                  reads=[("tmp", ti, 0), ("tmp", ti, 1)], writes=[("P", si)])

                    def back(h=h, qt=qt, idx=idx, last=last, si=si, ob=ob, vt=vt, vres=vres):
                        S.add("pe", lambda e: e.matmul(Ops[ob][0:65, :], lhsT=vt, rhs=Pt[si][:], start=(idx == 0), stop=(idx == last)),
                              reads=[("P", si), vres], writes=[("Ops", ob)])
                        if idx == last:
                            S.add("act", lambda e: e.copy(out=OsT[ob][:], in_=Ops[ob][0:65, :]), reads=[("Ops", ob)], writes=[("OsT", ob)])
                            for sb in range(4):
                                S.add("pe", lambda e, sb=sb: e.matmul(Bps[ob][:, sb, :], lhsT=OsT[ob][:, sb * 128:(sb + 1) * 128], rhs=identf[0:65, 0:65], start=True, stop=True),
                                      reads=[("OsT", ob), "identf"], writes=[("Bps", ob)])
                            S.add("dve", lambda e: e.tensor_copy(out=Osb[ob][:], in_=Bps[ob][:]), reads=[("Bps", ob)], writes=[("Osb", ob)])
                            S.add("dve", lambda e: e.reciprocal(out=rc[ob][:], in_=Osb[ob][:, :, 64:65]), reads=[("Osb", ob)], writes=[("rc", ob)])
                            for sb in range(4):
                                S.add("pool", lambda e, sb=sb: e.tensor_scalar(
                                    out=ocs[:, qt * 4 + sb, h * 64:(h + 1) * 64], in0=Osb[ob][:, sb, 0:64], scalar1=rc[ob][:, sb, 0:1], scalar2=None, op0=ALU.mult),
                                    reads=[("Osb", ob), ("rc", ob)], writes=[("ocs", h, qt)])

                    items.append((front, back))
        for k in range(len(items) + LA):
            if k < len(items):
                items[k][0]()
            if k >= LA:
                items[k - LA][1]()
        S.add("pool", lambda e: e.dma_start(out=oc.rearrange("(t p) d -> p t d", p=128), in_=ocs[:]),
              reads=[("ocs", h, qt) for h in range(8) for qt in range(4)], writes=["oc"], dma=True, key="oco")
        S.emit()
    return nc


def _perm(d):
    n = TL // d
    u = np.arange(TL)
    return (u % n) * d + u // n


def _t5_bucket(dist):
    df = np.maximum(dist, 1).astype(np.float32) / 16
    large = 16 + (np.log(df) / math.log(2048 / 16) * 16).astype(np.int32)
    return np.where(dist < 16, dist, np.minimum(large, 31))


def _stream_cols(s):
    if s < 4:
        return s * 64, 256 + (s // 2) * 64, 384 + (s // 2) * 64
    j = s - 4
    return 512 + j * 64, 1280 + j * 64, 2048 + j * 64


def _bias_tables(rel_bias, c):
    kj = np.arange(128)[:, None]
    qi = np.arange(128)[None, :]
    out = np.empty((128, NSTREAM, 2, 256), np.float32)
    for s in range(NSTREAM):
        d = _stream_dil(s)
        maxd = 127 if s < 4 else 128
        col = rel_bias[:, s]
        dp = 128 + qi - kj
        dc = qi - kj
        tp = np.where(dp <= maxd, col[_t5_bucket(dp * d)], np.float32(NEG)).astype(np.float32)
        tc = np.where(dc >= 0, col[_t5_bucket(np.maximum(dc, 0) * d)], np.float32(NEG)).astype(np.float32)
        out[:, s, 0, :128] = tp if c > 0 else np.float32(NEG)
        out[:, s, 1, :128] = tp
        out[:, s, 0, 128:] = tc
        out[:, s, 1, 128:] = tc
    return out


def k3_inputs(h, k2res, d):
    KTp = np.ascontiguousarray(np.concatenate([r["KT"] for r in k2res[:7]], axis=2))
    Vp = np.ascontiguousarray(np.concatenate([r["V"] for r in k2res[:7]], axis=0).reshape(112, 128, 8, 64).transpose(2, 1, 0, 3))
    p_ = np.arange(128)[:, None, None]
    m_ = np.arange(4)[None, :, None]
    j_ = np.arange(512)[None, None, :]
    mk = np.where(128 * m_ + p_ <= j_, np.float32(0), np.float32(NEG)).astype(np.float32)
    Kd, Vd, Qd = {}, {}, {}
    for s in range(NSTREAM):
        qc, kc, vc = _stream_cols(s)
        perm = _perm(_stream_dil(s))
        for c in range(NC):
            rows = c * TL + perm
            Qd[s, c] = h[rows, qc:qc + 64]
            Kd[s, c] = h[rows, kc:kc + 64].reshape(16, 128, 64)
            Vd[s, c] = h[rows, vc:vc + 64].reshape(16, 128, 64)
    maps = []
    for c in range(NC):
        QTb = np.empty((NSTREAM, 64, TL), h.dtype)
        KTb = np.empty((NSTREAM, 64, 16, 256), h.dtype)
        Vb = np.empty((NSTREAM, 128, 32, 64), h.dtype)
        for s in range(NSTREAM):
            per = 16 // _stream_dil(s)
            QTb[s] = Qd[s, c].T
            for b in range(16):
                if b % per:
                    kprev, vprev = Kd[s, c][b - 1], Vd[s, c][b - 1]
                elif c > 0:
                    bb = (b // per) * per + per - 1
                    kprev, vprev = Kd[s, c - 1][bb], Vd[s, c - 1][bb]
                else:
                    kprev, vprev = np.zeros((128, 64), h.dtype), np.zeros((128, 64), h.dtype)
                KTb[s, :, b, :128] = kprev.T
                KTb[s, :, b, 128:] = Kd[s, c][b].T
                Vb[s, :, 2 * b] = vprev
                Vb[s, :, 2 * b + 1] = Vd[s, c][b]
        vis = np.full((128, 7), NEG, np.float32)
        vis[:, :c] = 0.0
        maps.append({
            "qT": k2res[c]["qT"], "KTp": KTp, "KTo": k2res[c]["KT"], "Vp": Vp,
            "Vo": np.ascontiguousarray(k2res[c]["V"].reshape(16, 128, 8, 64).transpose(2, 1, 0, 3)),
            "vis": vis, "mk": mk, "QTb": QTb, "KTb": KTb, "Vb": Vb, "bT": _bias_tables(d["rel_bias"], c),
        })
    return maps


ALPHA = 8 ** 0.25
NE = 32


def build_k4():
    nc = bass.Bass("TRN2", target_bir_lowering=False)
    di = lambda n, s, t: nc.dram_tensor(n, s, t, kind="ExternalInput").ap()
    x = di("x", [TL, D], F32)
    Oa = di("Oa", [TL, 260], F32)
    Ob = di("Ob", [3, TL, 260], F32)
    oc = di("oc", [TL, 512], BF16)
    esk = di("esk", [128, 4], F32)
    wout = di("wout", [D, D], F32)
    lnp = di("lnp", [4, 128, D], F32)
    wr = di("wr", [D, 36], F32)
    br = di("br", [128, 36], F32)
    wg = di("wg", [NE, D, 256], F32)
    wu = di("wu", [NE, D, 256], F32)
    wd = di("wd", [NE, 256, D], F32)
    x2 = nc.dram_tensor("x2", [TL, D], F32, kind="ExternalOutput").ap()
    with contextlib.ExitStack() as stack:
        S = Sched(nc, stack)
        ld = lambda eng, dst, src, res, key: S.add(eng, lambda e: e.dma_start(out=dst, in_=src), writes=[res], dma=True, key=key)
        xs = S.sbuf("xs", [128, NT, D], F32)
        x1T = S.sbuf("x1T", [128, 8, TL], BF16)
        gate = S.sbuf("gate", [128, NT, NE], F32)
        lng = S.sbuf("lng", [128, D], F32)
        lnb = S.sbuf("lnb", [128, D], F32)
        esk_s = S.sbuf("esk_s", [128, 4], F32)
        br_s = S.sbuf("br_s", [128, 36], F32)
        wrf = S.sbuf("wrf", [128, 8, 36], F32)
        wr_b = S.sbuf("wr_b", [128, 8, 36], BF16)
        ident_f = S.sbuf("ident_f", [128, 128], F32)
        ident = S.sbuf("ident", [128, 128], BF16)
        epsc = S.sbuf("epsc", [128, 1], F32)
        wsf = S.sbuf("wsf", [128, 8, 512], F32)
        wdf = S.sbuf("wdf", [128, 2, D], F32)
        wout_b = S.sbuf("wout_b", [128, 8, D], BF16)
        wgu_b = [wout_b[:, :, 0:512], wout_b[:, :, 512:1024]]
        wd_b = [S.sbuf(f"wd_b{i}", [128, 2, D], BF16) for i in range(2)]
        Tps = [S.psum(f"Tps{i}", [128, 8, 128], BF16) for i in range(2)]
        Gps = [S.psum(f"Gps{i}", [128, 512], F32) for i in range(2)]
        Yps = [S.psum(f"Yps{i}", [128, D], F32) for i in range(2)]
        S.add("pool", lambda e: e.memset(ident_f[:], 1.0), writes=["ident_f"])
        S.add("pool", lambda e: e.affine_select(out=ident_f[:], in_=ident_f[:], pattern=[[-1, 128]], compare_op=ALU.is_equal, fill=0.0, base=0, channel_multiplier=1),
              reads=["ident_f"], writes=["ident_f"])
        S.add("dve", lambda e: e.tensor_copy(out=ident[:], in_=ident_f[:]), reads=["ident_f"], writes=["ident"])
        S.add("pool", lambda e: e.memset(epsc[:], 1e-5), writes=["epsc"])
        ld("sync", esk_s[:], esk, "esk", "esk")
        S.add("act", lambda e: e.activation(out=esk_s[:], in_=esk_s[:], func=AF.Exp), reads=["esk"], writes=["esk"])
        ld("sync", br_s[:], br, "br", "br")
        ld("sync", wrf[:], wr.rearrange("(c p) n -> p c n", p=128), "wrf", "wrf")
        S.add("dve", lambda e: e.tensor_copy(out=wr_b[:], in_=wrf[:]), reads=["wrf"], writes=["wr_b"])
        ld("sync", lng[:], lnp[0], "lng", "lng")
        ld("sync", lnb[:], lnp[1], "lnb", "lnb")
        woutv = wout.rearrange("(c p) n -> p c n", p=128)
        for half in range(2):
            ld("sync", wsf[:], woutv[:, :, half * 512:(half + 1) * 512], "wsf", "wsf")
            S.add("pool", lambda e, half=half: e.tensor_copy(out=wout_b[:, :, half * 512:(half + 1) * 512], in_=wsf[:]), reads=["wsf"], writes=[("wbig", half)])
        for tt in range(NT):
            ld("sync", xs[:, tt, :], x[tt * 128:(tt + 1) * 128, :], ("xs", tt), "xs")
        oas = [S.sbuf(f"oas{i}", [128, 4, 65], F32) for i in range(2)]
        obs = [S.sbuf(f"obs{i}", [128, 3, 260], F32) for i in range(2)]
        nbs = [S.sbuf(f"nbs{i}", [128, 4, 65], F32) for i in range(2)]
        obf = [S.sbuf(f"obf{i}", [128, D], BF16) for i in range(2)]
        oT = [S.sbuf(f"oT{i}", [128, 8, 128], BF16) for i in range(2)]
        xb = [S.sbuf(f"xb{i}", [128, D], BF16) for i in range(2)]
        junk = S.sbuf("junk", [128, D], F32)
        sm = [S.sbuf(f"sm{i}", [128, 64], F32) for i in range(2)]
        rt = [S.sbuf(f"rt{i}", [128, 256], F32) for i in range(2)]

        def layer_norm(tt, b, gres, bres):
            X = xs[:, tt, :]
            R = ("xs", tt)
            m = sm[b]
            S.add("dve", lambda e: e.reduce_sum(out=m[:, 0:1], in_=X, axis=AX.X), reads=[R], writes=[("sm", b, 0)])
            S.add("dve", lambda e: e.tensor_scalar(out=m[:, 1:2], in0=m[:, 0:1], scalar1=-1.0 / D, scalar2=None, op0=ALU.mult), reads=[("sm", b, 0)], writes=[("sm", b, 1)])
            S.add("act", lambda e: e.activation(out=X, in_=X, func=AF.Identity, bias=m[:, 1:2]), reads=[R, ("sm", b, 1)], writes=[R])
            S.add("pool", lambda e: e.memset(m[:, 2:3], 0.0), writes=[("sm", b, 2)])
            S.add("act", lambda e: e.activation(out=junk[:], in_=X, func=AF.Square, accum_out=m[:, 2:3]), reads=[R, ("sm", b, 2)], writes=["junk", ("sm", b, 2)])
            S.add("act", lambda e: e.activation(out=m[:, 3:4], in_=m[:, 2:3], func=AF.Sqrt, bias=epsc[:, 0:1], scale=1.0 / D), reads=[("sm", b, 2), "epsc"], writes=[("sm", b, 3)])
            S.add("dve", lambda e: e.reciprocal(out=m[:, 4:5], in_=m[:, 3:4]), reads=[("sm", b, 3)], writes=[("sm", b, 4)])
            S.add("dve", lambda e: e.scalar_tensor_tensor(out=X, in0=X, scalar=m[:, 4:5], in1=lng[:], op0=ALU.mult, op1=ALU.mult), reads=[R, ("sm", b, 4), gres], writes=[R])
            S.add("pool", lambda e: e.tensor_tensor(out=X, in0=X, in1=lnb[:], op=ALU.add), reads=[R, bres], writes=[R])

        Oav = Oa.rearrange("t (h d) -> t h d", d=65)
        for tt in range(NT):
            b = tt % 2
            ts_ = slice(tt * 128, (tt + 1) * 128)
            ld("sync", oas[b][:], Oav[ts_], ("oas", b), f"oas{b}")
            S.add("sync", lambda e, b=b, ts_=ts_: e.dma_start(out=obs[b][:], in_=Ob[:, ts_, :].rearrange("g t f -> t g f")), writes=[("obs", b)], dma=True, key=f"obs{b}")
            S.add("sync", lambda e, b=b, ts_=ts_: e.dma_start(out=obf[b][:, 512:1024], in_=oc[ts_, :]), writes=[("obf", b, 2)], dma=True, key=f"obfc{b}")
            m = sm[b]
            S.add("dve", lambda e, b=b, m=m: e.tensor_tensor(out=m[:, 8:12], in0=oas[b][:, :, 64], in1=esk_s[:], op=ALU.add), reads=[("oas", b), "esk"], writes=[("sm", b, 8)])
            S.add("dve", lambda e, m=m: e.reciprocal(out=m[:, 12:16], in_=m[:, 8:12]), reads=[("sm", b, 8)], writes=[("sm", b, 12)])
            for hh in range(4):
                S.add("pool", lambda e, b=b, m=m, hh=hh: e.tensor_scalar(out=obf[b][:, hh * 64:(hh + 1) * 64], in0=oas[b][:, hh, 0:64], scalar1=m[:, 12 + hh:13 + hh], scalar2=None, op0=ALU.mult),
                      reads=[("oas", b), ("sm", b, 12)], writes=[("obf", b, 0)])
            nbf = nbs[b][:].rearrange("p a b -> p (a b)")
            S.add("dve", lambda e, b=b, nbf=nbf: e.tensor_tensor(out=nbf, in0=obs[b][:, 0, :], in1=obs[b][:, 1, :], op=ALU.add), reads=[("obs", b)], writes=[("nbs", b)])
            S.add("dve", lambda e, b=b, nbf=nbf: e.tensor_tensor(out=nbf, in0=nbf, in1=obs[b][:, 2, :], op=ALU.add), reads=[("obs", b), ("nbs", b)], writes=[("nbs", b)])
            S.add("dve", lambda e, b=b, m=m: e.reciprocal(out=m[:, 16:20], in_=nbs[b][:, :, 64]), reads=[("nbs", b)], writes=[("sm", b, 16)])
            for hh in range(4):
                S.add("pool", lambda e, b=b, m=m, hh=hh: e.tensor_scalar(out=obf[b][:, 256 + hh * 64:256 + (hh + 1) * 64], in0=nbs[b][:, hh, 0:64], scalar1=m[:, 16 + hh:17 + hh], scalar2=None, op0=ALU.mult),
                      reads=[("nbs", b), ("sm", b, 16)], writes=[("obf", b, 1)])
            for kc in range(8):
                S.add("pe", lambda e, b=b, kc=kc: e.transpose(out=Tps[b][:, kc, :], in_=obf[b][:, kc * 128:(kc + 1) * 128], identity=ident[:]),
                      reads=[("obf", b, 0), ("obf", b, 1), ("obf", b, 2), "ident"], writes=[("Tps", b)])
            S.add("act", lambda e, b=b: e.copy(out=oT[b][:], in_=Tps[b][:]), reads=[("Tps", b)], writes=[("oT", b)])
            for half in range(2):
                for kc in range(8):
                    S.add("pe", lambda e, b=b, half=half, kc=kc: e.matmul(Yps[b][:, half * 512:(half + 1) * 512], lhsT=oT[b][:, kc, :], rhs=wout_b[:, kc, half * 512:(half + 1) * 512], start=(kc == 0), stop=(kc == 7)),
                          reads=[("oT", b), ("wbig", half)], writes=[("Yps", b)])
            S.add("dve", lambda e, b=b, tt=tt: e.scalar_tensor_tensor(out=xs[:, tt, :], in0=xs[:, tt, :], scalar=ALPHA, in1=Yps[b][:], op0=ALU.mult, op1=ALU.add),
                  reads=[("xs", tt), ("Yps", b)], writes=[("xs", tt)])
            layer_norm(tt, b, "lng", "lnb")
            S.add("pool", lambda e, b=b, tt=tt: e.tensor_copy(out=xb[b][:], in_=xs[:, tt, :]), reads=[("xs", tt)], writes=[("xb", b)])
            for kc in range(8):
                S.add("pe", lambda e, b=b, kc=kc: e.transpose(out=Tps[b][:, kc, :], in_=xb[b][:, kc * 128:(kc + 1) * 128], identity=ident[:]),
                      reads=[("xb", b), "ident"], writes=[("Tps", b)])
            S.add("act", lambda e, b=b, ts_=ts_: e.copy(out=x1T[:, :, ts_], in_=Tps[b][:]), reads=[("Tps", b)], writes=[("x1T", tt)])
            S.add("pool", lambda e, tt=tt: e.tensor_scalar(out=xs[:, tt, :], in0=xs[:, tt, :], scalar1=ALPHA, scalar2=None, op0=ALU.mult), reads=[("xs", tt)], writes=[("xs", tt)])
            for kc in range(8):
                S.add("pe", lambda e, b=b, kc=kc, ts_=ts_: e.matmul(Gps[b][:, 0:36], lhsT=x1T[:, kc, ts_], rhs=wr_b[:, kc, :], start=(kc == 0), stop=(kc == 7)),
                      reads=[("x1T", tt), "wr_b"], writes=[("Gps", b)])
            r = rt[b]
            L, E1, E2, q1, q2 = r[:, 0:36], r[:, 40:72], r[:, 72:104], r[:, 104:136], r[:, 136:168]
            RT = ("rt", b)
            dv = lambda fn, rd=(), wr_=(): S.add("dve", fn, reads=[RT, ("sm", b, 30)] + list(rd), writes=[RT, ("sm", b, 30)] + list(wr_))
            dv(lambda e, b=b, L=L: e.tensor_tensor(out=L, in0=Gps[b][:, 0:36], in1=br_s[:], op=ALU.add), rd=[("Gps", b), "br"])
            dv(lambda e, m=m, L=L: e.reduce_max(out=m[:, 30:31], in_=L[:, 0:4], axis=AX.X))
            dv(lambda e, m=m: e.tensor_scalar(out=m[:, 31:32], in0=m[:, 30:31], scalar1=-1.0, scalar2=None, op0=ALU.mult))
            S.add("pool", lambda e, m=m: e.memset(m[:, 36:37], 0.0), reads=[RT, ("sm", b, 30)], writes=[RT, ("sm", b, 30)])
            S.add("act", lambda e, m=m, L=L: e.activation(out=m[:, 32:36], in_=L[:, 0:4], func=AF.Exp, bias=m[:, 31:32], accum_out=m[:, 36:37]),
                  reads=[RT, ("sm", b, 30)], writes=[RT, ("sm", b, 30)])
            dv(lambda e, m=m: e.reciprocal(out=m[:, 37:38], in_=m[:, 36:37]))
            dv(lambda e, m=m, L=L: e.tensor_scalar(out=m[:, 40:44], in0=L[:, 0:4], scalar1=m[:, 30:31], scalar2=None, op0=ALU.is_equal))
            dv(lambda e, m=m: e.tensor_scalar(out=m[:, 44:48], in0=m[:, 40:44], scalar1=1e9, scalar2=-1e9, op0=ALU.mult, op1=ALU.add))
            for g in range(4):
                dv(lambda e, m=m, L=L, E1=E1, g=g: e.tensor_scalar(out=E1[:, 8 * g:8 * g + 8], in0=L[:, 4 + 8 * g:12 + 8 * g], scalar1=m[:, 44 + g:45 + g], scalar2=None, op0=ALU.add))
            dv(lambda e, m=m, E1=E1: e.reduce_max(out=m[:, 48:49], in_=E1, axis=AX.X))
            dv(lambda e, m=m, E1=E1, q1=q1: e.tensor_scalar(out=q1, in0=E1, scalar1=m[:, 48:49], scalar2=None, op0=ALU.is_equal))
            dv(lambda e, E1=E1, E2=E2, q1=q1: e.scalar_tensor_tensor(out=E2, in0=q1, scalar=-1e9, in1=E1, op0=ALU.mult, op1=ALU.add))
            dv(lambda e, m=m, E2=E2: e.reduce_max(out=m[:, 49:50], in_=E2, axis=AX.X))
            dv(lambda e, m=m, E2=E2, q2=q2: e.tensor_scalar(out=q2, in0=E2, scalar1=m[:, 49:50], scalar2=None, op0=ALU.is_equal))
            dv(lambda e, m=m: e.tensor_tensor(out=m[:, 50:51], in0=m[:, 49:50], in1=m[:, 48:49], op=ALU.subtract))
            S.add("act", lambda e, m=m: e.activation(out=m[:, 51:52], in_=m[:, 50:51], func=AF.Exp), reads=[RT, ("sm", b, 30)], writes=[RT, ("sm", b, 30)])
            dv(lambda e, m=m: e.tensor_scalar(out=m[:, 52:53], in0=m[:, 51:52], scalar1=1.0, scalar2=None, op0=ALU.add))
            dv(lambda e, m=m: e.reciprocal(out=m[:, 53:54], in_=m[:, 52:53]))
            dv(lambda e, m=m: e.tensor_tensor(out=m[:, 54:55], in0=m[:, 53:54], in1=m[:, 37:38], op=ALU.mult))
            dv(lambda e, m=m: e.tensor_tensor(out=m[:, 55:56], in0=m[:, 54:55], in1=m[:, 51:52], op=ALU.mult))
            dv(lambda e, m=m, q1=q1: e.tensor_scalar(out=q1, in0=q1, scalar1=m[:, 54:55], scalar2=None, op0=ALU.mult))
            dv(lambda e, m=m, q1=q1, q2=q2, tt=tt: e.scalar_tensor_tensor(out=gate[:, tt, :], in0=q2, scalar=m[:, 55:56], in1=q1, op0=ALU.mult, op1=ALU.add), wr_=[("gate", tt)])
        ld("sync", lng[:], lnp[2], "lng", "lng")
        ld("sync", lnb[:], lnp[3], "lnb", "lnb")
        sg = [S.sbuf(f"sg{i}", [128, 256], F32) for i in range(2)]
        hb_ = [S.sbuf(f"hb{i}", [128, 256], BF16) for i in range(2)]
        hT = [S.sbuf(f"hT{i}", [128, 2, 128], BF16) for i in range(2)]
        stages = []
        cnt = 0
        for ex in range(NE):
            eb = ex % 2
            for tt in range(NT):
                b = cnt % 2
                cnt += 1
                ts_ = slice(tt * 128, (tt + 1) * 128)

                def stA(ex=ex, eb=eb, tt=tt, b=b, ts_=ts_):
                    if tt == 0:
                        S.add("sync", lambda e: e.dma_start(out=wsf[:, :, 0:256], in_=wg[ex].rearrange("(c p) n -> p c n", p=128)), writes=[("wsf", 0), "wsf"], dma=True, key="wsf0")
                        ld("sync", wsf[:, :, 256:512], wu[ex].rearrange("(c p) n -> p c n", p=128), ("wsf", 1), "wsf1")
                        ld("sync", wdf[:], wd[ex].rearrange("(c p) n -> p c n", p=128), "wdf", "wdf")
                        S.add("pool", lambda e: e.tensor_copy(out=wgu_b[eb], in_=wsf[:]), reads=["wsf", ("wsf", 0), ("wsf", 1)], writes=[("wbig", eb)])
                        S.add("pool", lambda e: e.tensor_copy(out=wd_b[eb][:], in_=wdf[:]), reads=["wdf"], writes=[("wd_b", eb)])
                    for kc in range(8):
                        S.add("pe", lambda e, kc=kc: e.matmul(Gps[b][:], lhsT=x1T[:, kc, ts_], rhs=wgu_b[eb][:, kc, :], start=(kc == 0), stop=(kc == 7)),
                              reads=[("x1T", tt), ("wbig", eb)], writes=[("Gps", b)])
                    S.add("act", lambda e: e.activation(out=sg[b][:], in_=Gps[b][:, 0:256], func=AF.Silu), reads=[("Gps", b)], writes=[("sg", b)])
                    S.add("dve", lambda e: e.scalar_tensor_tensor(out=hb_[b][:], in0=Gps[b][:, 256:512], scalar=gate[:, tt, ex:ex + 1], in1=sg[b][:], op0=ALU.mult, op1=ALU.mult),
                          reads=[("Gps", b), ("sg", b), ("gate", tt)], writes=[("hb", b)])

                def stB(b=b):
                    for dc in range(2):
                        S.add("pe", lambda e, dc=dc: e.transpose(out=Tps[b][:, dc, :], in_=hb_[b][:, dc * 128:(dc + 1) * 128], identity=ident[:]),
                              reads=[("hb", b), "ident"], writes=[("Tps", b)])
                    S.add("act", lambda e: e.copy(out=hT[b][:], in_=Tps[b][:, 0:2, :]), reads=[("Tps", b)], writes=[("hT", b)])

                def stC(eb=eb, tt=tt, b=b):
                    for half in range(2):
                        for dc in range(2):
                            S.add("pe", lambda e, half=half, dc=dc: e.matmul(Yps[b][:, half * 512:(half + 1) * 512], lhsT=hT[b][:, dc, :], rhs=wd_b[eb][:, dc, half * 512:(half + 1) * 512], start=(dc == 0), stop=(dc == 1)),
                                  reads=[("hT", b), ("wd_b", eb)], writes=[("Yps", b)])
                    S.add("dve", lambda e: e.tensor_tensor(out=xs[:, tt, :], in0=xs[:, tt, :], in1=Yps[b][:], op=ALU.add),
                          reads=[("xs", tt), ("Yps", b)], writes=[("xs", tt)])

                stages.append((stA, stB, stC))
        n_st = len(stages)
        for k in range(n_st + 2):
            if k < n_st:
                stages[k][0]()
            if 1 <= k <= n_st:
                stages[k - 1][1]()
            if k >= 2:
                stages[k - 2][2]()
        for tt in range(NT):
            layer_norm(tt, tt % 2, "lng", "lnb")
            S.add("pool", lambda e, tt=tt: e.dma_start(out=x2[tt * 128:(tt + 1) * 128, :], in_=xs[:, tt, :]), reads=[("xs", tt)], writes=[("x2", tt)], dma=True, key="x2o")
        S.emit()
    return nc


def k4_inputs(xcur, k3res, d, l):
    bc = lambda v: np.ascontiguousarray(np.broadcast_to(np.asarray(v, np.float32).reshape(1, -1), (128, v.size)))
    lnp = np.ascontiguousarray(np.stack([bc(d["ln1_g"][l]), bc(d["ln1_b"][l]), bc(d["ln2_g"][l]), bc(d["ln2_b"][l])], 0))
    wr = np.ascontiguousarray(np.concatenate([d["w_route_group"][l], d["w_route_expert"][l].transpose(1, 0, 2).reshape(D, 32)], axis=1))
    br = bc(np.concatenate([d["b_route_group"][l], d["b_route_expert"][l].reshape(32)]))
    perms = [_perm(_stream_dil(s)) for s in range(NSTREAM)]
    maps = []
    for c in range(NC):
        O = k3res[c]["Oab"]
        nat = np.empty_like(O)
        for s in range(NSTREAM):
            nat[s][perms[s]] = O[s]
        maps.append({
            "x": np.ascontiguousarray(xcur[c * TL:(c + 1) * TL]),
            "Oa": np.ascontiguousarray(nat[:4].transpose(1, 0, 2).reshape(TL, 260)),
            "Ob": np.ascontiguousarray(nat[4:].reshape(3, 4, TL, 65).transpose(0, 2, 1, 3).reshape(3, TL, 260)),
            "oc": k3res[c]["oc"], "esk": bc(d["sinks"][l]), "wout": d["w_out"][l], "lnp": lnp, "wr": wr, "br": br,
            "wg": d["w_expert_gate"][l], "wu": d["w_expert_up"][l], "wd": d["w_expert_down"][l],
        })
    return maps


_PROGS = {}


def _prog(name, fn):
    if name not in _PROGS:
        _PROGS[name] = fn()
    return _PROGS[name]


def kernel(**inputs):
    d = {k: np.asarray(v) for k, v in inputs.items()}
    x = np.ascontiguousarray(d["x"][0], dtype=np.float32)
    for l in range(4):
        r1 = _run(_prog("k1", build_k1), [{"xT": np.ascontiguousarray(x[c * TL:(c + 1) * TL].T), "w": d["w_in"][l]} for c in range(NC)])
        h = np.concatenate([r["h"] for r in r1], axis=0)
        r2 = _run(_prog("k2", build_k2), k2_inputs(h, d, l))
        r3 = _run(_prog("k3", build_k3), k3_inputs(h, r2, d))
        r4 = _run(_prog("k4", build_k4), k4_inputs(x, r3, d, l))
        x = np.concatenate([r["x2"] for r in r4], axis=0)
    return x[None].astype(np.float32)
```

```python
import contextlib
import math
import numpy as np
import concourse.bass as bass
import concourse.mybir as mybir
from concourse.bass_utils import run_bass_kernel_spmd

F32 = mybir.dt.float32
BF16 = mybir.dt.bfloat16
I32 = mybir.dt.int32
AF = mybir.ActivationFunctionType
ALU = mybir.AluOpType
AX = mybir.AxisListType

EPOCH = 30000
COMPUTE = ("act", "dve", "pool", "pe")


class _Op:
    __slots__ = ("eng", "fn", "deps", "dma", "key", "seq", "val")


class Sched:
    def __init__(self, nc, stack):
        self.nc = nc
        self.stack = stack
        self.ops = []
        self.lastw = {}
        self.readers = {}
        self.nseq = {e: 0 for e in ("sync", "act", "dve", "pool", "pe")}
        self.dmacount = {}
        self.n_tensors = 0

    def sbuf(self, name, shape, dtype):
        return self.stack.enter_context(self.nc.sbuf_tensor(name, list(shape), dtype))

    def psum(self, name, shape, dtype):
        return self.stack.enter_context(self.nc.psum_tensor(name, list(shape), dtype))

    def add(self, eng, fn, reads=(), writes=(), dma=False, key=None):
        op = _Op()
        op.eng, op.fn, op.dma = eng, fn, dma
        idx = len(self.ops)
        deps = {}
        for r in reads:
            w = self.lastw.get(r)
            if w is not None:
                deps[w] = "raw"
        for w_ in writes:
            w = self.lastw.get(w_)
            if w is not None and w not in deps:
                deps[w] = "waw"
            for rd in self.readers.get(w_, ()):
                if rd not in deps:
                    deps[rd] = "war"
        op.deps = deps
        if dma:
            if key is None:
                r0 = writes[0]
                key = r0[0] if isinstance(r0, tuple) else r0
            op.key = key
            c = self.dmacount.get(key, 0) + 1
            self.dmacount[key] = c
            op.val = 16 * c
        else:
            op.key = None
            self.nseq[eng] += 1
            op.seq = self.nseq[eng]
        for w_ in writes:
            self.lastw[w_] = idx
            self.readers[w_] = []
        for r in reads:
            self.readers.setdefault(r, []).append(idx)
        self.ops.append(op)
        return idx

    def emit(self):
        nc, stack = self.nc, self.stack
        csem = {}
        for e in COMPUTE:
            n_ep = self.nseq[e] // EPOCH + 1
            csem[e] = [stack.enter_context(nc.semaphore(f"c_{e}_{i}")) for i in range(n_ep)]
        dsem = {k: stack.enter_context(nc.semaphore(f"d_{i}")) for i, k in enumerate(self.dmacount)}
        per_eng = {e: [] for e in self.nseq}
        waited = {e: {} for e in self.nseq}
        dmaseen = {k: 0 for k in self.dmacount}
        plan = []
        for op in self.ops:
            need = {}
            for d, kind in op.deps.items():
                dop = self.ops[d]
                if dop.dma:
                    s = dsem[dop.key]
                    v = dmaseen[dop.key]
                    assert v >= dop.val
                else:
                    if dop.eng == op.eng and not op.dma:
                        if op.eng == "pe" or kind != "raw":
                            continue
                    ep = (dop.seq - 1) // EPOCH
                    s = csem[dop.eng][ep]
                    v = dop.seq - ep * EPOCH
                if need.get(s, (0,))[0] < v:
                    need[s] = (v, s)
            waits = []
            wd = waited[op.eng]
            for s, (v, _) in need.items():
                if wd.get(s, 0) >= v:
                    continue
                wd[s] = v
                waits.append((s, v))
            if op.dma:
                dmaseen[op.key] = op.val
                inc = (dsem[op.key], 16)
            else:
                ep = (op.seq - 1) // EPOCH
                inc = (csem[op.eng][ep], 1)
            per_eng[op.eng].append((op.fn, waits, inc))
        self.per_eng = per_eng
        final_waits = [(dsem[k], 16 * c) for k, c in self.dmacount.items()]

        def run(engname, eng):
            for fn, waits, inc in per_eng[engname]:
                for s, v in waits:
                    eng.wait_ge(s, v)
                ins = fn(eng)
                ins.then_inc(inc[0], inc[1])

        with nc.Block() as block:
            @block.sync
            def _(e):
                run("sync", e)
                for s, v in final_waits:
                    e.wait_ge(s, v)

            @block.scalar
            def _(e):
                run("act", e)

            @block.vector
            def _(e):
                run("dve", e)

            @block.gpsimd
            def _(e):
                run("pool", e)

            @block.tensor
            def _(e):
                run("pe", e)


NC = 8
TL = 2048
NT = 16
D = 1024
N_IN = 3232
NEG = -30000.0


def _run(nc, in_maps):
    res = run_bass_kernel_spmd(nc, in_maps, core_ids=list(range(NC)))
    return res.results


def build_k1():
    nc = bass.Bass("TRN2", target_bir_lowering=False)
    xT = nc.dram_tensor("xT", [D, TL], F32, kind="ExternalInput").ap()
    w = nc.dram_tensor("w", [D, N_IN], F32, kind="ExternalInput").ap()
    h = nc.dram_tensor("h", [TL, N_IN], BF16, kind="ExternalOutput").ap()
    with contextlib.ExitStack() as stack:
        S = Sched(nc, stack)
        xf = [S.sbuf(f"xf{i}", [128, TL], F32) for i in range(2)]
        xb = S.sbuf("xb", [128, 8, TL], BF16)
        for kc in range(8):
            b = kc % 2
            S.add("sync", lambda e, kc=kc, b=b: e.dma_start(out=xf[b][:], in_=xT[kc * 128:(kc + 1) * 128, :]),
                  writes=[("xf", b)], dma=True, key=f"xf{b}")
            S.add("dve" if b else "pool", lambda e, kc=kc, b=b: e.tensor_copy(out=xb[:, kc, :], in_=xf[b][:]),
                  reads=[("xf", b)], writes=[("xb", kc)])
        wf = [S.sbuf(f"wf{i}", [128, 8, 512], F32) for i in range(2)]
        wb = [S.sbuf(f"wb{i}", [128, 8, 512], BF16) for i in range(2)]
        ps = [S.psum(f"ps{i}", [128, 512], F32) for i in range(4)]
        hs = [S.sbuf(f"hs{i}", [128, 512], BF16) for i in range(4)]
        wv = w.rearrange("(c p) n -> p c n", p=128)
        for cb in range(7):
            ncol = 512 if cb < 6 else N_IN - 6 * 512
            c0 = cb * 512
            b = cb % 2
            S.add("sync", lambda e, b=b, c0=c0, ncol=ncol: e.dma_start(out=wf[b][:, :, :ncol], in_=wv[:, :, c0:c0 + ncol]),
                  writes=[("wf", b)], dma=True, key=f"wf{b}")
            S.add("pool", lambda e, b=b, ncol=ncol: e.tensor_copy(out=wb[b][:, :, :ncol], in_=wf[b][:, :, :ncol]),
                  reads=[("wf", b)], writes=[("wb", b)])
            for tt in range(NT):
                i = (cb * NT + tt) % 4
                for kc in range(8):
                    S.add("pe", lambda e, i=i, kc=kc, tt=tt, b=b, ncol=ncol: e.matmul(
                        ps[i][:, :ncol], lhsT=xb[:, kc, tt * 128:(tt + 1) * 128], rhs=wb[b][:, kc, :ncol],
                        start=(kc == 0), stop=(kc == 7)),
                        reads=[("xb", kc), ("wb", b)], writes=[("ps", i)])
                S.add("act" if i % 2 else "dve", lambda e, i=i, ncol=ncol: (e.copy if i % 2 else e.tensor_copy)(
                    out=hs[i][:, :ncol], in_=ps[i][:, :ncol]), reads=[("ps", i)], writes=[("hs", i)])
                S.add("pool", lambda e, i=i, tt=tt, c0=c0, ncol=ncol: e.dma_start(
                    out=h[tt * 128:(tt + 1) * 128, c0:c0 + ncol], in_=hs[i][:, :ncol]),
                    reads=[("hs", i)], writes=[("h", cb, tt)], dma=True, key=f"hso{i}")
        S.emit()
    return nc


def build_k2():
    nc = bass.Bass("TRN2", target_bir_lowering=False)
    di = lambda n, s, t: nc.dram_tensor(n, s, t, kind="ExternalInput").ap()
    cqT = di("cqT", [256, TL], BF16)
    ckvT = di("ckvT", [128, TL], BF16)
    kr2 = di("kr2", [2, 32, TL], BF16)
    gq = di("gq", [128, 2], F32)
    gkv = di("gkv", [128, 1], F32)
    wuq = di("wuq", [256, 768], F32)
    wuqs = di("wuqs", [256, 768], F32)
    wk = di("wk", [128, 512], F32)
    wv = di("wv", [128, 512], F32)
    cs2 = di("cs2", [2, 32, TL], F32)
    qT = nc.dram_tensor("qT", [8, 96, TL], BF16, kind="ExternalOutput").ap()
    KT = nc.dram_tensor("KT", [8, 96, TL], BF16, kind="ExternalOutput").ap()
    V = nc.dram_tensor("V", [TL, 512], BF16, kind="ExternalOutput").ap()
    with contextlib.ExitStack() as stack:
        S = Sched(nc, stack)
        cq = S.sbuf("cq", [128, 2, TL], BF16)
        ckv = S.sbuf("ckv", [128, TL], BF16)
        kr = S.sbuf("kr", [96, TL], BF16)
        krs = S.sbuf("krs", [96, TL], BF16)
        C2 = S.sbuf("C2", [96, TL], F32)
        S2 = S.sbuf("S2", [96, TL], F32)
        gq_s = S.sbuf("gq_s", [128, 2], F32)
        gkv_s = S.sbuf("gkv_s", [128, 1], F32)
        wf = S.sbuf("wf", [128, 2, 768], F32)
        wuq_b = S.sbuf("wuq_b", [128, 2, 768], BF16)
        wuqs_b = S.sbuf("wuqs_b", [128, 2, 768], BF16)
        wkf = S.sbuf("wkf", [128, 512], F32)
        wk_b = S.sbuf("wk_b", [128, 512], BF16)
        wv_b = S.sbuf("wv_b", [128, 512], BF16)
        ones = S.sbuf("ones", [128, 128], F32)
        sq = S.sbuf("sq", [128, TL], F32)
        rq = S.sbuf("rq", [128, TL], F32)
        rk = S.sbuf("rk", [128, TL], F32)
        cqn = S.sbuf("cqn", [128, 2, TL], BF16)
        ckvn = S.sbuf("ckvn", [128, TL], BF16)
        krr = S.sbuf("krr", [96, TL], BF16)
        t1 = S.sbuf("t1", [96, TL], F32)
        t2 = S.sbuf("t2", [96, TL], F32)
        ld = lambda dst, src, res, key: S.add("sync", lambda e: e.dma_start(out=dst, in_=src), writes=[res], dma=True, key=key)
        ld(cq[:], cqT.rearrange("(c p) n -> p c n", p=128), "cq", "cq")
        ld(ckv[:], ckvT, "ckv", "ckv")
        ld(kr[64:96, :], kr2[0], "kr", "kr")
        ld(krs[64:96, :], kr2[1], "krs", "krs")
        ld(C2[64:96, :], cs2[0], "C2", "C2")
        ld(S2[64:96, :], cs2[1], "S2", "S2")
        ld(gq_s[:], gq, "gq", "gq")
        ld(gkv_s[:], gkv, "gkv", "gkv")
        S.add("pool", lambda e: e.memset(ones[:], 1.0), writes=["ones"])
        epsc = S.sbuf("epsc", [128, 1], F32)
        S.add("pool", lambda e: e.memset(epsc[:], 1e-6), writes=["epsc"])
        ld(wf[:], wuq.rearrange("(c p) n -> p c n", p=128), "wf", "wf")
        S.add("dve", lambda e: e.tensor_copy(out=wuq_b[:], in_=wf[:]), reads=["wf"], writes=["wuq_b"])
        ld(wf[:], wuqs.rearrange("(c p) n -> p c n", p=128), "wf", "wf")
        S.add("dve", lambda e: e.tensor_copy(out=wuqs_b[:], in_=wf[:]), reads=["wf"], writes=["wuqs_b"])
        ld(wkf[:], wk, "wkf", "wkf")
        S.add("dve", lambda e: e.tensor_copy(out=wk_b[:], in_=wkf[:]), reads=["wkf"], writes=["wk_b"])
        ld(wkf[:], wv, "wkf", "wkf")
        S.add("dve", lambda e: e.tensor_copy(out=wv_b[:], in_=wkf[:]), reads=["wkf"], writes=["wv_b"])
        ps = [S.psum(f"ps{i}", [128, 512], F32) for i in range(6)]
        pi = [0]

        def nps():
            pi[0] = (pi[0] + 1) % 6
            return pi[0]

        def rstd(src_chunks, nfeat, eps, out_r, tag):
            nchunk = len(src_chunks)
            for g4 in range(4):
                cs = slice(g4 * 512, (g4 + 1) * 512)
                i = nps()
                for kc, (src, res) in enumerate(src_chunks):
                    S.add("dve", lambda e, src=src, cs=cs: e.tensor_tensor(out=sq[:, cs], in0=src[:, cs], in1=src[:, cs], op=ALU.mult),
                          reads=[res], writes=[("sq", g4)])
                    S.add("pe", lambda e, i=i, cs=cs, kc=kc: e.matmul(ps[i][:], lhsT=ones[:], rhs=sq[:, cs], start=(kc == 0), stop=(kc == nchunk - 1)),
                          reads=["ones", ("sq", g4)], writes=[("ps", i)])
                S.add("act", lambda e, i=i, cs=cs: e.activation(out=out_r[:, cs], in_=ps[i][:], func=AF.Sqrt, bias=epsc[:, 0:1], scale=1.0 / nfeat),
                      reads=[("ps", i), "epsc"], writes=[(tag, g4)])
                S.add("dve", lambda e, cs=cs: e.reciprocal(out=out_r[:, cs], in_=out_r[:, cs]),
                      reads=[(tag, g4)], writes=[(tag, g4)])

        rstd([(cq[:, 0, :], "cq"), (cq[:, 1, :], "cq")], 256, 1e-6, rq, "rq")
        for kc in range(2):
            for g4 in range(4):
                cs = slice(g4 * 512, (g4 + 1) * 512)
                S.add("dve", lambda e, kc=kc, cs=cs: e.scalar_tensor_tensor(out=cqn[:, kc, cs], in0=cq[:, kc, cs], scalar=gq_s[:, kc:kc + 1], in1=rq[:, cs], op0=ALU.mult, op1=ALU.mult),
                      reads=["cq", "gq", ("rq", g4)], writes=[("cqn", kc, g4)])
        rstd([(ckv, "ckv")], 128, 1e-6, rk, "rk")
        for g4 in range(4):
            cs = slice(g4 * 512, (g4 + 1) * 512)
            S.add("dve", lambda e, cs=cs: e.scalar_tensor_tensor(out=ckvn[:, cs], in0=ckv[:, cs], scalar=gkv_s[:, 0:1], in1=rk[:, cs], op0=ALU.mult, op1=ALU.mult),
                  reads=["ckv", "gkv", ("rk", g4)], writes=[("ckvn", g4)])
        S.add("dve", lambda e: e.tensor_tensor(out=t1[64:96, :], in0=kr[64:96, :], in1=C2[64:96, :], op=ALU.mult), reads=["kr", "C2"], writes=["t1"])
        S.add("pool", lambda e: e.tensor_tensor(out=t2[64:96, :], in0=krs[64:96, :], in1=S2[64:96, :], op=ALU.mult), reads=["krs", "S2"], writes=["t2"])
        S.add("dve", lambda e: e.tensor_tensor(out=krr[64:96, :], in0=t1[64:96, :], in1=t2[64:96, :], op=ALU.add), reads=["t1", "t2"], writes=["krr"])
        for h in range(8):
            S.add("pool", lambda e, h=h: e.dma_start(out=KT[h, 64:96, :], in_=krr[64:96, :]), reads=["krr"], writes=[("KTr", h)], dma=True, key="krro")
        qs = [S.sbuf(f"qs{i}", [96, 512], BF16) for i in range(2)]
        ks = [S.sbuf(f"ks{i}", [64, 512], BF16) for i in range(2)]
        u1 = [S.sbuf(f"u1{i}", [96, 512], F32) for i in range(2)]
        u2 = [S.sbuf(f"u2{i}", [96, 512], F32) for i in range(2)]
        n = 0
        for h in range(8):
            for g4 in range(4):
                cs = slice(g4 * 512, (g4 + 1) * 512)
                b = n % 2
                n += 1
                ia, ib, ik = nps(), nps(), nps()
                for kc in range(2):
                    S.add("pe", lambda e, ia=ia, kc=kc, h=h, cs=cs: e.matmul(ps[ia][0:96, :], lhsT=wuq_b[:, kc, h * 96:(h + 1) * 96], rhs=cqn[:, kc, cs], start=(kc == 0), stop=(kc == 1)),
                          reads=["wuq_b", ("cqn", kc, g4)], writes=[("ps", ia)])
                for kc in range(2):
                    S.add("pe", lambda e, ib=ib, kc=kc, h=h, cs=cs: e.matmul(ps[ib][0:96, :], lhsT=wuqs_b[:, kc, h * 96:(h + 1) * 96], rhs=cqn[:, kc, cs], start=(kc == 0), stop=(kc == 1)),
                          reads=["wuqs_b", ("cqn", kc, g4)], writes=[("ps", ib)])
                S.add("pe", lambda e, ik=ik, h=h, cs=cs: e.matmul(ps[ik][0:64, :], lhsT=wk_b[:, h * 64:(h + 1) * 64], rhs=ckvn[:, cs], start=True, stop=True),
                      reads=["wk_b", ("ckvn", g4)], writes=[("ps", ik)])
                S.add("act", lambda e, ia=ia, b=b: e.copy(out=qs[b][0:64, :], in_=ps[ia][0:64, :]), reads=[("ps", ia)], writes=[("qs", b, 0)])
                S.add("dve", lambda e, ia=ia, b=b, cs=cs: e.tensor_tensor(out=u1[b][64:96, :], in0=ps[ia][64:96, :], in1=C2[64:96, cs], op=ALU.mult),
                      reads=[("ps", ia), "C2"], writes=[("u1", b)])
                S.add("dve", lambda e, ib=ib, b=b, cs=cs: e.tensor_tensor(out=u2[b][64:96, :], in0=ps[ib][64:96, :], in1=S2[64:96, cs], op=ALU.mult),
                      reads=[("ps", ib), "S2"], writes=[("u2", b)])
                S.add("pool", lambda e, b=b: e.tensor_tensor(out=qs[b][64:96, :], in0=u1[b][64:96, :], in1=u2[b][64:96, :], op=ALU.add),
                      reads=[("u1", b), ("u2", b)], writes=[("qs", b, 1)])
                S.add("act", lambda e, ik=ik, b=b: e.copy(out=ks[b][:], in_=ps[ik][0:64, :]), reads=[("ps", ik)], writes=[("ks", b)])
                S.add("sync", lambda e, b=b, h=h, cs=cs: e.dma_start(out=qT[h, :, cs], in_=qs[b][:]), reads=[("qs", b, 0), ("qs", b, 1)], writes=[("qT", h, g4)], dma=True, key=f"qso{b}")
                S.add("sync", lambda e, b=b, h=h, cs=cs: e.dma_start(out=KT[h, 0:64, cs], in_=ks[b][:]), reads=[("ks", b)], writes=[("KTn", h, g4)], dma=True, key=f"kso{b}")
        vs = [S.sbuf(f"vs{i}", [128, 512], BF16) for i in range(2)]
        for tt in range(NT):
            b = tt % 2
            i = nps()
            S.add("pe", lambda e, i=i, tt=tt: e.matmul(ps[i][:], lhsT=ckvn[:, tt * 128:(tt + 1) * 128], rhs=wv_b[:], start=True, stop=True),
                  reads=["wv_b", ("ckvn", tt // 4)], writes=[("ps", i)])
            S.add("act", lambda e, i=i, b=b: e.copy(out=vs[b][:], in_=ps[i][:]), reads=[("ps", i)], writes=[("vs", b)])
            S.add("sync", lambda e, b=b, tt=tt: e.dma_start(out=V[tt * 128:(tt + 1) * 128, :], in_=vs[b][:]), reads=[("vs", b)], writes=[("V", tt)], dma=True, key=f"vso{b}")
        S.emit()
    return nc


def _rope_tables(c):
    inv = (10000.0 ** (-np.arange(0, 32, 2, dtype=np.float32) / 32)).astype(np.float32)
    pos = np.arange(c * TL, (c + 1) * TL, dtype=np.float32)
    ang = (pos[:, None] * inv[None, :]).astype(np.float32)
    cos, sin = np.cos(ang).astype(np.float32).T, np.sin(ang).astype(np.float32).T
    return np.ascontiguousarray(np.stack([np.concatenate([cos, cos], 0), np.concatenate([-sin, sin], 0)], 0))


def k2_inputs(h, d, l):
    wuq = d["w_uq"][l]
    wuqs = np.zeros_like(wuq)
    for hh in range(8):
        b = hh * 96 + 64
        wuqs[:, b:b + 16] = wuq[:, b + 16:b + 32]
        wuqs[:, b + 16:b + 32] = wuq[:, b:b + 16]
    wukv = d["w_ukv"][l].reshape(128, 8, 128)
    wk = np.ascontiguousarray(wukv[:, :, :64].reshape(128, 512))
    wv = np.ascontiguousarray(wukv[:, :, 64:].reshape(128, 512))
    gq = np.ascontiguousarray(d["mla_q_norm"][l].reshape(2, 128).T)
    gkv = np.ascontiguousarray(d["mla_kv_norm"][l].reshape(128, 1))
    maps = []
    for c in range(NC):
        hc = h[c * TL:(c + 1) * TL]
        krT = hc[:, 3200:3232].T
        maps.append({
            "cqT": np.ascontiguousarray(hc[:, 2816:3072].T),
            "ckvT": np.ascontiguousarray(hc[:, 3072:3200].T),
            "kr2": np.ascontiguousarray(np.stack([krT, np.concatenate([krT[16:], krT[:16]], 0)], 0)),
            "gq": gq, "gkv": gkv, "wuq": wuq, "wuqs": wuqs, "wk": wk, "wv": wv,
            "cs2": _rope_tables(c),
        })
    return maps


SCALE_C = 96 ** -0.5
NSTREAM = 16


def _stream_dil(s):
    return 1 if s < 4 else (1, 4, 16)[(s - 4) // 4]


def build_k3():
    nc = bass.Bass("TRN2", target_bir_lowering=False)
    di = lambda n, s, t: nc.dram_tensor(n, s, t, kind="ExternalInput").ap()
    qT = di("qT", [8, 96, TL], BF16)
    KTa = di("KTa", [8, 96, 8 * TL], BF16)
    Va = di("Va", [8, 128, 128, 64], BF16)
    mkc = di("mkc", [128, 32, 512], BF16)
    QTb = di("QTb", [NSTREAM, 64, TL], BF16)
    KTb = di("KTb", [NSTREAM, 64, 16, 256], BF16)
    Vb = di("Vb", [NSTREAM, 128, 32, 64], BF16)
    bT = di("bT", [128, NSTREAM, 2, 256], F32)
    oc = nc.dram_tensor("oc", [TL, 512], BF16, kind="ExternalOutput").ap()
    Oab = nc.dram_tensor("Oab", [NSTREAM, TL, 65], F32, kind="ExternalOutput").ap()
    with contextlib.ExitStack() as stack:
        S = Sched(nc, stack)
        mk_s = S.sbuf("mk_s", [128, 32, 512], BF16)
        bT_s = S.sbuf("bT_s", [128, NSTREAM, 2, 256], F32)
        ld = lambda eng, dst, src, res, key: S.add(eng, lambda e: e.dma_start(out=dst, in_=src), writes=[res], dma=True, key=key)
        ld("sync", mk_s[:], mkc, "mk", "mk")
        ld("sync", bT_s[:], bT, "bT", "bT")
        Sps = [S.psum(f"Sps{i}", [128, 512], F32) for i in range(4)]
        Ops = [S.psum(f"Ops{i}", [128, 512], F32) for i in range(2)]
        Bps = [S.psum(f"Bps{i}", [128, 4, 65], F32) for i in range(2)]
        Pt = [S.sbuf(f"Pt{i}", [128, 512], BF16) for i in range(4)]
        tmp = [S.sbuf(f"tmp{i}", [128, 512], F32) for i in range(2)]
        qb = [S.sbuf(f"qb{i}", [64, TL], BF16) for i in range(2)]
        kb = [S.sbuf(f"kb{i}", [64, 16, 256], BF16) for i in range(2)]
        vb = [S.sbuf(f"vb{i}", [128, 32, 65], BF16) for i in range(2)]
        obs = [S.sbuf(f"obs{i}", [128, 16, 65], F32) for i in range(2)]
        for i in range(2):
            S.add("pool", lambda e, i=i: e.memset(vb[i][:], 1.0), writes=[("vb", i)])
        LA = 2
        items = []
        cnt = 0
        for s in range(NSTREAM):
            sb_ = s % 2
            d = _stream_dil(s)
            per = 16 // d
            for pr in range(8):
                si = cnt % 4
                ti = cnt % 2
                bi = cnt % 2
                cnt += 1

                def front(s=s, sb_=sb_, per=per, pr=pr, si=si, ti=ti):
                    if pr == 0:
                        ld("sync", qb[sb_][:], QTb[s], ("qb", sb_), f"qb{sb_}")
                        ld("sync", kb[sb_][:], KTb[s], ("kb", sb_), f"kb{sb_}")
                        S.add("sync", lambda e: e.dma_start(out=vb[sb_][:, :, 0:64], in_=Vb[s]), reads=[("vb", sb_)], writes=[("vb", sb_)], dma=True, key=f"vb{sb_}")
                    for j in range(2):
                        b = pr * 2 + j
                        for half in range(2):
                            S.add("pe", lambda e, j=j, half=half, b=b: e.matmul(
                                Sps[si][:, j * 256 + half * 128: j * 256 + half * 128 + 128], lhsT=kb[sb_][:, b, half * 128:(half + 1) * 128],
                                rhs=qb[sb_][:, b * 128:(b + 1) * 128], start=True, stop=True),
                                reads=[("kb", sb_), ("qb", sb_)], writes=[("S", si)])
                    for j in range(2):
                        b = pr * 2 + j
                        var = 0 if (b % per) == 0 else 1
                        S.add("dve", lambda e, j=j, var=var: e.scalar_tensor_tensor(
                            out=tmp[ti][:, j * 256:(j + 1) * 256], in0=Sps[si][:, j * 256:(j + 1) * 256], scalar=0.125,
                            in1=bT_s[:, s, var, :], op0=ALU.mult, op1=ALU.add),
                            reads=[("S", si), "bT"], writes=[("tmp", ti, j)])
                    S.add("act", lambda e: e.activation(out=Pt[si][:], in_=tmp[ti][:], func=AF.Exp),
                          reads=[("tmp", ti, 0), ("tmp", ti, 1)], writes=[("P", si)])

                def back(s=s, sb_=sb_, pr=pr, si=si, bi=bi):
                    for j in range(2):
                        b = pr * 2 + j
                        for half in range(2):
                            S.add("pe", lambda e, j=j, half=half, b=b: e.matmul(
                                Bps[bi][:, j, :], lhsT=Pt[si][:, j * 256 + half * 128: j * 256 + half * 128 + 128],
                                rhs=vb[sb_][:, b * 2 + half, :], start=(half == 0), stop=(half == 1)),
                                reads=[("P", si), ("vb", sb_)], writes=[("Bps", bi)])
                    S.add("dve", lambda e: e.tensor_copy(out=obs[sb_][:, pr * 2:pr * 2 + 2, :], in_=Bps[bi][:, 0:2, :]),
                          reads=[("Bps", bi)], writes=[("obs", sb_)])
                    if pr == 7:
                        S.add("pool", lambda e: e.dma_start(out=Oab[s].rearrange("(b p) d -> p b d", p=128), in_=obs[sb_][:]),
                              reads=[("obs", sb_)], writes=[("Oab", s)], dma=True, key=f"obso{sb_}")

                items.append((front, back))
        kall = S.sbuf("kall", [96, 8 * TL], BF16)
        vall = S.sbuf("vall", [128, 128, 65], BF16)
        qh = [S.sbuf(f"qh{i}", [96, TL], BF16) for i in range(2)]
        OsT = [S.sbuf(f"OsT{i}", [65, 512], F32) for i in range(2)]
        Osb = [S.sbuf(f"Osb{i}", [128, 4, 65], F32) for i in range(2)]
        rc = [S.sbuf(f"rc{i}", [128, 4, 1], F32) for i in range(2)]
        ocs = S.sbuf("ocs", [128, NT, 512], BF16)
        identf = S.sbuf("identf", [128, 128], F32)
        S.add("pool", lambda e: e.memset(identf[:], 1.0), writes=["identf"])
        S.add("pool", lambda e: e.affine_select(out=identf[:], in_=identf[:], pattern=[[-1, 128]], compare_op=ALU.is_equal, fill=0.0, base=0, channel_multiplier=1),
              reads=["identf"], writes=["identf"])
        S.add("pool", lambda e: e.memset(vall[:], 1.0), writes=[("vq", q) for q in range(4)])

        def load_quarter(h, q):
            ks = slice(q * 4096, (q + 1) * 4096)
            S.add("sync", lambda e: e.dma_start(out=kall[:, ks], in_=KTa[h, :, ks]), writes=[("kq", q)], dma=True, key=f"kq{q}")
            S.add("sync", lambda e: e.dma_start(out=vall[:, q * 32:(q + 1) * 32, 0:64], in_=Va[h, :, q * 32:(q + 1) * 32, :]), reads=[("vq", q)], writes=[("vq", q)], dma=True, key=f"vq{q}")

        ocnt = 0
        item_groups = [items]
        for h in range(8):
            hb = h % 2
            items = []
            item_groups.append(items)
            for g in (3, 2, 1, 0):
                ob = ocnt % 2
                ocnt += 1
                ntile = 32 * (g + 1)
                last = ntile - 1
                qs_ = slice(g * 512, (g + 1) * 512)
                for idx in range(ntile):
                    si = cnt % 4
                    ti = cnt % 2
                    cnt += 1
                    z = idx - 32 * g

                    def front(h=h, hb=hb, g=g, idx=idx, z=z, si=si, ti=ti, qs_=qs_):
                        if g == 3 and idx == 0:
                            ld("sync", qh[hb][:], qT[h], ("qh", hb), f"qh{hb}")
                            if h == 0:
                                for q in range(4):
                                    load_quarter(0, q)
                            else:
                                load_quarter(h, 0)
                        if idx == 3 and g < 3 and h < 7:
                            load_quarter(h + 1, g + 1)
                        S.add("pe", lambda e: e.matmul(Sps[si][:], lhsT=kall[:, idx * 128:(idx + 1) * 128], rhs=qh[hb][:, qs_], start=True, stop=True),
                              reads=[("kq", idx // 32), ("qh", hb)], writes=[("S", si)])
                        if z < 0:
                            S.add("act", lambda e: e.activation(out=Pt[si][:], in_=Sps[si][:], func=AF.Exp, scale=SCALE_C),
                                  reads=[("S", si)], writes=[("P", si)])
                        else:
                            S.add("dve", lambda e: e.scalar_tensor_tensor(out=tmp[ti][:], in0=Sps[si][:], scalar=SCALE_C, in1=mk_s[:, z, :], op0=ALU.mult, op1=ALU.add),
                                  reads=[("S", si), "mk"], writes=[("tmp", ti, 0), ("tmp", ti, 1)])
                            S.add("act", lambda e: e.activation(out=Pt[si][:], in_=tmp[ti][:], func=AF.Exp),
                                  reads=[("tmp", ti, 0), ("tmp", ti, 1)], writes=[("P", si)])

                    def back(h=h, g=g, idx=idx, last=last, si=si, ob=ob):
                        S.add("pe", lambda e: e.matmul(Ops[ob][0:65, :], lhsT=vall[:, idx, :], rhs=Pt[si][:], start=(idx == 0), stop=(idx == last)),
                              reads=[("P", si), ("vq", idx // 32)], writes=[("Ops", ob)])
                        if idx == last:
                            S.add("act", lambda e: e.copy(out=OsT[ob][:], in_=Ops[ob][0:65, :]), reads=[("Ops", ob)], writes=[("OsT", ob)])
                            for sb in range(4):
                                S.add("pe", lambda e, sb=sb: e.matmul(Bps[ob][:, sb, :], lhsT=OsT[ob][:, sb * 128:(sb + 1) * 128], rhs=identf[0:65, 0:65], start=True, stop=True),
                                      reads=[("OsT", ob), "identf"], writes=[("Bps", ob)])
                            S.add("dve", lambda e: e.tensor_copy(out=Osb[ob][:], in_=Bps[ob][:]), reads=[("Bps", ob)], writes=[("Osb", ob)])
                            S.add("dve", lambda e: e.reciprocal(out=rc[ob][:], in_=Osb[ob][:, :, 64:65]), reads=[("Osb", ob)], writes=[("rc", ob)])
                            for sb in range(4):
                                S.add("pool", lambda e, sb=sb: e.tensor_scalar(
                                    out=ocs[:, g * 4 + sb, h * 64:(h + 1) * 64], in0=Osb[ob][:, sb, 0:64], scalar1=rc[ob][:, sb, 0:1], scalar2=None, op0=ALU.mult),
                                    reads=[("Osb", ob), ("rc", ob)], writes=[("ocs", h, g)])

                    items.append((front, back))
        for items in item_groups:
            for k in range(len(items) + LA):
                if k < len(items):
                    items[k][0]()
                if k >= LA:
                    items[k - LA][1]()
        S.add("pool", lambda e: e.dma_start(out=oc.rearrange("(t p) d -> p t d", p=128), in_=ocs[:]),
              reads=[("ocs", h, qt) for h in range(8) for qt in range(4)], writes=["oc"], dma=True, key="oco")
        S.emit()
    return nc


def _perm(d):
    n = TL // d
    u = np.arange(TL)
    return (u % n) * d + u // n


def _t5_bucket(dist):
    df = np.maximum(dist, 1).astype(np.float32) / 16
    large = 16 + (np.log(df) / math.log(2048 / 16) * 16).astype(np.int32)
    return np.where(dist < 16, dist, np.minimum(large, 31))


def _stream_cols(s):
    if s < 4:
        return s * 64, 256 + (s // 2) * 64, 384 + (s // 2) * 64
    j = s - 4
    return 512 + j * 64, 1280 + j * 64, 2048 + j * 64


def _bias_tables(rel_bias, c):
    kj = np.arange(128)[:, None]
    qi = np.arange(128)[None, :]
    out = np.empty((128, NSTREAM, 2, 256), np.float32)
    for s in range(NSTREAM):
        d = _stream_dil(s)
        maxd = 127 if s < 4 else 128
        col = rel_bias[:, s]
        dp = 128 + qi - kj
        dc = qi - kj
        tp = np.where(dp <= maxd, col[_t5_bucket(dp * d)], np.float32(NEG)).astype(np.float32)
        tc = np.where(dc >= 0, col[_t5_bucket(np.maximum(dc, 0) * d)], np.float32(NEG)).astype(np.float32)
        out[:, s, 0, :128] = tp if c > 0 else np.float32(NEG)
        out[:, s, 1, :128] = tp
        out[:, s, 0, 128:] = tc
        out[:, s, 1, 128:] = tc
    return out


def k3_inputs(h, k2res, d):
    KTa = np.ascontiguousarray(np.concatenate([r["KT"] for r in k2res], axis=2))
    Va = np.ascontiguousarray(np.concatenate([r["V"] for r in k2res], axis=0).reshape(128, 128, 8, 64).transpose(2, 1, 0, 3))
    qTa = np.concatenate([r["qT"] for r in k2res], axis=2).reshape(8, 96, 128, 128)
    p_ = np.arange(128)[:, None, None, None]
    z_ = np.arange(32)[None, :, None, None]
    j_ = np.arange(4)[None, None, :, None]
    i_ = np.arange(128)[None, None, None, :]
    Kd, Vd, Qd = {}, {}, {}
    for s in range(NSTREAM):
        qc, kc, vc = _stream_cols(s)
        perm = _perm(_stream_dil(s))
        for c in range(NC):
            rows = c * TL + perm
            Qd[s, c] = h[rows, qc:qc + 64]
            Kd[s, c] = h[rows, kc:kc + 64].reshape(16, 128, 64)
            Vd[s, c] = h[rows, vc:vc + 64].reshape(16, 128, 64)
    maps = []
    for c in range(NC):
        QTb = np.empty((NSTREAM, 64, TL), h.dtype)
        KTb = np.empty((NSTREAM, 64, 16, 256), h.dtype)
        Vb = np.empty((NSTREAM, 128, 32, 64), h.dtype)
        for s in range(NSTREAM):
            per = 16 // _stream_dil(s)
            QTb[s] = Qd[s, c].T
            for b in range(16):
                if b % per:
                    kprev, vprev = Kd[s, c][b - 1], Vd[s, c][b - 1]
                elif c > 0:
                    bb = (b // per) * per + per - 1
                    kprev, vprev = Kd[s, c - 1][bb], Vd[s, c - 1][bb]
                else:
                    kprev, vprev = np.zeros((128, 64), h.dtype), np.zeros((128, 64), h.dtype)
                KTb[s, :, b, :128] = kprev.T
                KTb[s, :, b, 128:] = Kd[s, c][b].T
                Vb[s, :, 2 * b] = vprev
                Vb[s, :, 2 * b + 1] = Vd[s, c][b]
        vis_ = (z_ < c + 8 * j_) | ((z_ == c + 8 * j_) & (p_ <= i_))
        mkc = np.where(vis_, np.float32(0), np.float32(NEG)).astype(h.dtype).reshape(128, 32, 512)
        maps.append({
            "qT": np.ascontiguousarray(qTa[:, :, c::8, :].reshape(8, 96, TL)), "KTa": KTa, "Va": Va, "mkc": mkc,
            "QTb": QTb, "KTb": KTb, "Vb": Vb, "bT": _bias_tables(d["rel_bias"], c),
        })
    return maps


ALPHA = 8 ** 0.25
NE = 32


def build_k4():
    nc = bass.Bass("TRN2", target_bir_lowering=False)
    di = lambda n, s, t: nc.dram_tensor(n, s, t, kind="ExternalInput").ap()
    x = di("x", [TL, D], F32)
    Oa = di("Oa", [TL, 260], F32)
    Ob = di("Ob", [3, TL, 260], F32)
    oc = di("oc", [TL, 512], BF16)
    esk = di("esk", [128, 4], F32)
    wout = di("wout", [D, D], F32)
    lnp = di("lnp", [4, 128, D], F32)
    wr = di("wr", [D, 36], F32)
    br = di("br", [128, 36], F32)
    wg = di("wg", [NE, D, 256], F32)
    wu = di("wu", [NE, D, 256], F32)
    wd = di("wd", [NE, 256, D], F32)
    x2 = nc.dram_tensor("x2", [TL, D], F32, kind="ExternalOutput").ap()
    with contextlib.ExitStack() as stack:
        S = Sched(nc, stack)
        ld = lambda eng, dst, src, res, key: S.add(eng, lambda e: e.dma_start(out=dst, in_=src), writes=[res], dma=True, key=key)
        xs = S.sbuf("xs", [128, NT, D], F32)
        x1T = S.sbuf("x1T", [128, 8, TL], BF16)
        gate = S.sbuf("gate", [128, NT, NE], F32)
        lng = S.sbuf("lng", [128, D], F32)
        lnb = S.sbuf("lnb", [128, D], F32)
        esk_s = S.sbuf("esk_s", [128, 4], F32)
        br_s = S.sbuf("br_s", [128, 36], F32)
        wrf = S.sbuf("wrf", [128, 8, 36], F32)
        wr_b = S.sbuf("wr_b", [128, 8, 36], BF16)
        ident_f = S.sbuf("ident_f", [128, 128], F32)
        ident = S.sbuf("ident", [128, 128], BF16)
        epsc = S.sbuf("epsc", [128, 1], F32)
        wsf = S.sbuf("wsf", [128, 8, 512], F32)
        wdf = S.sbuf("wdf", [128, 2, D], F32)
        wout_b = S.sbuf("wout_b", [128, 8, D], BF16)
        wgu_b = [wout_b[:, :, 0:512], wout_b[:, :, 512:1024]]
        wd_b = [S.sbuf(f"wd_b{i}", [128, 2, D], BF16) for i in range(2)]
        Tps = [S.psum(f"Tps{i}", [128, 8, 128], BF16) for i in range(2)]
        Gps = [S.psum(f"Gps{i}", [128, 512], F32) for i in range(2)]
        Yps = [S.psum(f"Yps{i}", [128, D], F32) for i in range(2)]
        S.add("pool", lambda e: e.memset(ident_f[:], 1.0), writes=["ident_f"])
        S.add("pool", lambda e: e.affine_select(out=ident_f[:], in_=ident_f[:], pattern=[[-1, 128]], compare_op=ALU.is_equal, fill=0.0, base=0, channel_multiplier=1),
              reads=["ident_f"], writes=["ident_f"])
        S.add("dve", lambda e: e.tensor_copy(out=ident[:], in_=ident_f[:]), reads=["ident_f"], writes=["ident"])
        S.add("pool", lambda e: e.memset(epsc[:], 1e-5), writes=["epsc"])
        ld("sync", esk_s[:], esk, "esk", "esk")
        S.add("act", lambda e: e.activation(out=esk_s[:], in_=esk_s[:], func=AF.Exp), reads=["esk"], writes=["esk"])
        ld("sync", br_s[:], br, "br", "br")
        ld("sync", wrf[:], wr.rearrange("(c p) n -> p c n", p=128), "wrf", "wrf")
        S.add("dve", lambda e: e.tensor_copy(out=wr_b[:], in_=wrf[:]), reads=["wrf"], writes=["wr_b"])
        ld("sync", lng[:], lnp[0], "lng", "lng")
        ld("sync", lnb[:], lnp[1], "lnb", "lnb")
        woutv = wout.rearrange("(c p) n -> p c n", p=128)
        for half in range(2):
            ld("sync", wsf[:], woutv[:, :, half * 512:(half + 1) * 512], "wsf", "wsf")
            S.add("pool", lambda e, half=half: e.tensor_copy(out=wout_b[:, :, half * 512:(half + 1) * 512], in_=wsf[:]), reads=["wsf"], writes=[("wbig", half)])
        for tt in range(NT):
            ld("sync", xs[:, tt, :], x[tt * 128:(tt + 1) * 128, :], ("xs", tt), "xs")
        oas = [S.sbuf(f"oas{i}", [128, 4, 65], F32) for i in range(2)]
        obs = [S.sbuf(f"obs{i}", [128, 3, 260], F32) for i in range(2)]
        nbs = [S.sbuf(f"nbs{i}", [128, 4, 65], F32) for i in range(2)]
        obf = [S.sbuf(f"obf{i}", [128, D], BF16) for i in range(2)]
        oT = [S.sbuf(f"oT{i}", [128, 8, 128], BF16) for i in range(2)]
        xb = [S.sbuf(f"xb{i}", [128, D], BF16) for i in range(2)]
        junk = S.sbuf("junk", [128, D], F32)
        sm = [S.sbuf(f"sm{i}", [128, 64], F32) for i in range(2)]
        rt = [S.sbuf(f"rt{i}", [128, 256], F32) for i in range(2)]

        def layer_norm(tt, b, gres, bres):
            X = xs[:, tt, :]
            R = ("xs", tt)
            m = sm[b]
            S.add("dve", lambda e: e.reduce_sum(out=m[:, 0:1], in_=X, axis=AX.X), reads=[R], writes=[("sm", b, 0)])
            S.add("dve", lambda e: e.tensor_scalar(out=m[:, 1:2], in0=m[:, 0:1], scalar1=-1.0 / D, scalar2=None, op0=ALU.mult), reads=[("sm", b, 0)], writes=[("sm", b, 1)])
            S.add("act", lambda e: e.activation(out=X, in_=X, func=AF.Identity, bias=m[:, 1:2]), reads=[R, ("sm", b, 1)], writes=[R])
            S.add("pool", lambda e: e.memset(m[:, 2:3], 0.0), writes=[("sm", b, 2)])
            S.add("act", lambda e: e.activation(out=junk[:], in_=X, func=AF.Square, accum_out=m[:, 2:3]), reads=[R, ("sm", b, 2)], writes=["junk", ("sm", b, 2)])
            S.add("act", lambda e: e.activation(out=m[:, 3:4], in_=m[:, 2:3], func=AF.Sqrt, bias=epsc[:, 0:1], scale=1.0 / D), reads=[("sm", b, 2), "epsc"], writes=[("sm", b, 3)])
            S.add("dve", lambda e: e.reciprocal(out=m[:, 4:5], in_=m[:, 3:4]), reads=[("sm", b, 3)], writes=[("sm", b, 4)])
            S.add("dve", lambda e: e.scalar_tensor_tensor(out=X, in0=X, scalar=m[:, 4:5], in1=lng[:], op0=ALU.mult, op1=ALU.mult), reads=[R, ("sm", b, 4), gres], writes=[R])
            S.add("pool", lambda e: e.tensor_tensor(out=X, in0=X, in1=lnb[:], op=ALU.add), reads=[R, bres], writes=[R])

        Oav = Oa.rearrange("t (h d) -> t h d", d=65)
        for tt in range(NT):
            b = tt % 2
            ts_ = slice(tt * 128, (tt + 1) * 128)
            ld("sync", oas[b][:], Oav[ts_], ("oas", b), f"oas{b}")
            S.add("sync", lambda e, b=b, ts_=ts_: e.dma_start(out=obs[b][:], in_=Ob[:, ts_, :].rearrange("g t f -> t g f")), writes=[("obs", b)], dma=True, key=f"obs{b}")
            S.add("sync", lambda e, b=b, ts_=ts_: e.dma_start(out=obf[b][:, 512:1024], in_=oc[ts_, :]), writes=[("obf", b, 2)], dma=True, key=f"obfc{b}")
            m = sm[b]
            S.add("dve", lambda e, b=b, m=m: e.tensor_tensor(out=m[:, 8:12], in0=oas[b][:, :, 64], in1=esk_s[:], op=ALU.add), reads=[("oas", b), "esk"], writes=[("sm", b, 8)])
            S.add("dve", lambda e, m=m: e.reciprocal(out=m[:, 12:16], in_=m[:, 8:12]), reads=[("sm", b, 8)], writes=[("sm", b, 12)])
            for hh in range(4):
                S.add("pool", lambda e, b=b, m=m, hh=hh: e.tensor_scalar(out=obf[b][:, hh * 64:(hh + 1) * 64], in0=oas[b][:, hh, 0:64], scalar1=m[:, 12 + hh:13 + hh], scalar2=None, op0=ALU.mult),
                      reads=[("oas", b), ("sm", b, 12)], writes=[("obf", b, 0)])
            nbf = nbs[b][:].rearrange("p a b -> p (a b)")
            S.add("dve", lambda e, b=b, nbf=nbf: e.tensor_tensor(out=nbf, in0=obs[b][:, 0, :], in1=obs[b][:, 1, :], op=ALU.add), reads=[("obs", b)], writes=[("nbs", b)])
            S.add("dve", lambda e, b=b, nbf=nbf: e.tensor_tensor(out=nbf, in0=nbf, in1=obs[b][:, 2, :], op=ALU.add), reads=[("obs", b), ("nbs", b)], writes=[("nbs", b)])
            S.add("dve", lambda e, b=b, m=m: e.reciprocal(out=m[:, 16:20], in_=nbs[b][:, :, 64]), reads=[("nbs", b)], writes=[("sm", b, 16)])
            for hh in range(4):
                S.add("pool", lambda e, b=b, m=m, hh=hh: e.tensor_scalar(out=obf[b][:, 256 + hh * 64:256 + (hh + 1) * 64], in0=nbs[b][:, hh, 0:64], scalar1=m[:, 16 + hh:17 + hh], scalar2=None, op0=ALU.mult),
                      reads=[("nbs", b), ("sm", b, 16)], writes=[("obf", b, 1)])
            for kc in range(8):
                S.add("pe", lambda e, b=b, kc=kc: e.transpose(out=Tps[b][:, kc, :], in_=obf[b][:, kc * 128:(kc + 1) * 128], identity=ident[:]),
                      reads=[("obf", b, 0), ("obf", b, 1), ("obf", b, 2), "ident"], writes=[("Tps", b)])
            S.add("act", lambda e, b=b: e.copy(out=oT[b][:], in_=Tps[b][:]), reads=[("Tps", b)], writes=[("oT", b)])
            for half in range(2):
                for kc in range(8):
                    S.add("pe", lambda e, b=b, half=half, kc=kc: e.matmul(Yps[b][:, half * 512:(half + 1) * 512], lhsT=oT[b][:, kc, :], rhs=wout_b[:, kc, half * 512:(half + 1) * 512], start=(kc == 0), stop=(kc == 7)),
                          reads=[("oT", b), ("wbig", half)], writes=[("Yps", b)])
            S.add("dve", lambda e, b=b, tt=tt: e.scalar_tensor_tensor(out=xs[:, tt, :], in0=xs[:, tt, :], scalar=ALPHA, in1=Yps[b][:], op0=ALU.mult, op1=ALU.add),
                  reads=[("xs", tt), ("Yps", b)], writes=[("xs", tt)])
            layer_norm(tt, b, "lng", "lnb")
            S.add("pool", lambda e, b=b, tt=tt: e.tensor_copy(out=xb[b][:], in_=xs[:, tt, :]), reads=[("xs", tt)], writes=[("xb", b)])
            for kc in range(8):
                S.add("pe", lambda e, b=b, kc=kc: e.transpose(out=Tps[b][:, kc, :], in_=xb[b][:, kc * 128:(kc + 1) * 128], identity=ident[:]),
                      reads=[("xb", b), "ident"], writes=[("Tps", b)])
            S.add("act", lambda e, b=b, ts_=ts_: e.copy(out=x1T[:, :, ts_], in_=Tps[b][:]), reads=[("Tps", b)], writes=[("x1T", tt)])
            S.add("pool", lambda e, tt=tt: e.tensor_scalar(out=xs[:, tt, :], in0=xs[:, tt, :], scalar1=ALPHA, scalar2=None, op0=ALU.mult), reads=[("xs", tt)], writes=[("xs", tt)])
            for kc in range(8):
                S.add("pe", lambda e, b=b, kc=kc, ts_=ts_: e.matmul(Gps[b][:, 0:36], lhsT=x1T[:, kc, ts_], rhs=wr_b[:, kc, :], start=(kc == 0), stop=(kc == 7)),
                      reads=[("x1T", tt), "wr_b"], writes=[("Gps", b)])
            r = rt[b]
            L, E1, E2, q1, q2 = r[:, 0:36], r[:, 40:72], r[:, 72:104], r[:, 104:136], r[:, 136:168]
            RT = ("rt", b)
            dv = lambda fn, rd=(), wr_=(): S.add("dve", fn, reads=[RT, ("sm", b, 30)] + list(rd), writes=[RT, ("sm", b, 30)] + list(wr_))
            dv(lambda e, b=b, L=L: e.tensor_tensor(out=L, in0=Gps[b][:, 0:36], in1=br_s[:], op=ALU.add), rd=[("Gps", b), "br"])
            dv(lambda e, m=m, L=L: e.reduce_max(out=m[:, 30:31], in_=L[:, 0:4], axis=AX.X))
            dv(lambda e, m=m: e.tensor_scalar(out=m[:, 31:32], in0=m[:, 30:31], scalar1=-1.0, scalar2=None, op0=ALU.mult))
            S.add("pool", lambda e, m=m: e.memset(m[:, 36:37], 0.0), reads=[RT, ("sm", b, 30)], writes=[RT, ("sm", b, 30)])
            S.add("act", lambda e, m=m, L=L: e.activation(out=m[:, 32:36], in_=L[:, 0:4], func=AF.Exp, bias=m[:, 31:32], accum_out=m[:, 36:37]),
                  reads=[RT, ("sm", b, 30)], writes=[RT, ("sm", b, 30)])
            dv(lambda e, m=m: e.reciprocal(out=m[:, 37:38], in_=m[:, 36:37]))
            dv(lambda e, m=m, L=L: e.tensor_scalar(out=m[:, 40:44], in0=L[:, 0:4], scalar1=m[:, 30:31], scalar2=None, op0=ALU.is_equal))
            dv(lambda e, m=m: e.tensor_scalar(out=m[:, 44:48], in0=m[:, 40:44], scalar1=1e9, scalar2=-1e9, op0=ALU.mult, op1=ALU.add))
            for g in range(4):
                dv(lambda e, m=m, L=L, E1=E1, g=g: e.tensor_scalar(out=E1[:, 8 * g:8 * g + 8], in0=L[:, 4 + 8 * g:12 + 8 * g], scalar1=m[:, 44 + g:45 + g], scalar2=None, op0=ALU.add))
            dv(lambda e, m=m, E1=E1: e.reduce_max(out=m[:, 48:49], in_=E1, axis=AX.X))
            dv(lambda e, m=m, E1=E1, q1=q1: e.tensor_scalar(out=q1, in0=E1, scalar1=m[:, 48:49], scalar2=None, op0=ALU.is_equal))
            dv(lambda e, E1=E1, E2=E2, q1=q1: e.scalar_tensor_tensor(out=E2, in0=q1, scalar=-1e9, in1=E1, op0=ALU.mult, op1=ALU.add))
            dv(lambda e, m=m, E2=E2: e.reduce_max(out=m[:, 49:50], in_=E2, axis=AX.X))
            dv(lambda e, m=m, E2=E2, q2=q2: e.tensor_scalar(out=q2, in0=E2, scalar1=m[:, 49:50], scalar2=None, op0=ALU.is_equal))
            dv(lambda e, m=m: e.tensor_tensor(out=m[:, 50:51], in0=m[:, 49:50], in1=m[:, 48:49], op=ALU.subtract))
            S.add("act", lambda e, m=m: e.activation(out=m[:, 51:52], in_=m[:, 50:51], func=AF.Exp), reads=[RT, ("sm", b, 30)], writes=[RT, ("sm", b, 30)])
            dv(lambda e, m=m: e.tensor_scalar(out=m[:, 52:53], in0=m[:, 51:52], scalar1=1.0, scalar2=None, op0=ALU.add))
            dv(lambda e, m=m: e.reciprocal(out=m[:, 53:54], in_=m[:, 52:53]))
            dv(lambda e, m=m: e.tensor_tensor(out=m[:, 54:55], in0=m[:, 53:54], in1=m[:, 37:38], op=ALU.mult))
            dv(lambda e, m=m: e.tensor_tensor(out=m[:, 55:56], in0=m[:, 54:55], in1=m[:, 51:52], op=ALU.mult))
            dv(lambda e, m=m, q1=q1: e.tensor_scalar(out=q1, in0=q1, scalar1=m[:, 54:55], scalar2=None, op0=ALU.mult))
            dv(lambda e, m=m, q1=q1, q2=q2, tt=tt: e.scalar_tensor_tensor(out=gate[:, tt, :], in0=q2, scalar=m[:, 55:56], in1=q1, op0=ALU.mult, op1=ALU.add), wr_=[("gate", tt)])
        ld("sync", lng[:], lnp[2], "lng", "lng")
        ld("sync", lnb[:], lnp[3], "lnb", "lnb")
        sg = [S.sbuf(f"sg{i}", [128, 256], F32) for i in range(2)]
        hb_ = [S.sbuf(f"hb{i}", [128, 256], BF16) for i in range(2)]
        hT = [S.sbuf(f"hT{i}", [128, 2, 128], BF16) for i in range(2)]
        stages = []
        cnt = 0
        for ex in range(NE):
            eb = ex % 2
            for tt in range(NT):
                b = cnt % 2
                cnt += 1
                ts_ = slice(tt * 128, (tt + 1) * 128)

                def stA(ex=ex, eb=eb, tt=tt, b=b, ts_=ts_):
                    if tt == 0:
                        S.add("sync", lambda e: e.dma_start(out=wsf[:, :, 0:256], in_=wg[ex].rearrange("(c p) n -> p c n", p=128)), writes=[("wsf", 0), "wsf"], dma=True, key="wsf0")
                        ld("sync", wsf[:, :, 256:512], wu[ex].rearrange("(c p) n -> p c n", p=128), ("wsf", 1), "wsf1")
                        ld("sync", wdf[:], wd[ex].rearrange("(c p) n -> p c n", p=128), "wdf", "wdf")
                        S.add("pool", lambda e: e.tensor_copy(out=wgu_b[eb], in_=wsf[:]), reads=["wsf", ("wsf", 0), ("wsf", 1)], writes=[("wbig", eb)])
                        S.add("pool", lambda e: e.tensor_copy(out=wd_b[eb][:], in_=wdf[:]), reads=["wdf"], writes=[("wd_b", eb)])
                    for kc in range(8):
                        S.add("pe", lambda e, kc=kc: e.matmul(Gps[b][:], lhsT=x1T[:, kc, ts_], rhs=wgu_b[eb][:, kc, :], start=(kc == 0), stop=(kc == 7)),
                              reads=[("x1T", tt), ("wbig", eb)], writes=[("Gps", b)])
                    S.add("act", lambda e: e.activation(out=sg[b][:], in_=Gps[b][:, 0:256], func=AF.Silu), reads=[("Gps", b)], writes=[("sg", b)])
                    S.add("dve", lambda e: e.scalar_tensor_tensor(out=hb_[b][:], in0=Gps[b][:, 256:512], scalar=gate[:, tt, ex:ex + 1], in1=sg[b][:], op0=ALU.mult, op1=ALU.mult),
                          reads=[("Gps", b), ("sg", b), ("gate", tt)], writes=[("hb", b)])

                def stB(b=b):
                    for dc in range(2):
                        S.add("pe", lambda e, dc=dc: e.transpose(out=Tps[b][:, dc, :], in_=hb_[b][:, dc * 128:(dc + 1) * 128], identity=ident[:]),
                              reads=[("hb", b), "ident"], writes=[("Tps", b)])
                    S.add("act", lambda e: e.copy(out=hT[b][:], in_=Tps[b][:, 0:2, :]), reads=[("Tps", b)], writes=[("hT", b)])

                def stC(eb=eb, tt=tt, b=b):
                    for half in range(2):
                        for dc in range(2):
                            S.add("pe", lambda e, half=half, dc=dc: e.matmul(Yps[b][:, half * 512:(half + 1) * 512], lhsT=hT[b][:, dc, :], rhs=wd_b[eb][:, dc, half * 512:(half + 1) * 512], start=(dc == 0), stop=(dc == 1)),
                                  reads=[("hT", b), ("wd_b", eb)], writes=[("Yps", b)])
                    S.add("dve", lambda e: e.tensor_tensor(out=xs[:, tt, :], in0=xs[:, tt, :], in1=Yps[b][:], op=ALU.add),
                          reads=[("xs", tt), ("Yps", b)], writes=[("xs", tt)])

                stages.append((stA, stB, stC))
        n_st = len(stages)
        for k in range(n_st + 2):
            if k < n_st:
                stages[k][0]()
            if 1 <= k <= n_st:
                stages[k - 1][1]()
            if k >= 2:
                stages[k - 2][2]()
        for tt in range(NT):
            layer_norm(tt, tt % 2, "lng", "lnb")
            S.add("pool", lambda e, tt=tt: e.dma_start(out=x2[tt * 128:(tt + 1) * 128, :], in_=xs[:, tt, :]), reads=[("xs", tt)], writes=[("x2", tt)], dma=True, key="x2o")
        S.emit()
    return nc


def k4_inputs(xcur, k3res, d, l):
    bc = lambda v: np.ascontiguousarray(np.broadcast_to(np.asarray(v, np.float32).reshape(1, -1), (128, v.size)))
    lnp = np.ascontiguousarray(np.stack([bc(d["ln1_g"][l]), bc(d["ln1_b"][l]), bc(d["ln2_g"][l]), bc(d["ln2_b"][l])], 0))
    wr = np.ascontiguousarray(np.concatenate([d["w_route_group"][l], d["w_route_expert"][l].transpose(1, 0, 2).reshape(D, 32)], axis=1))
    br = bc(np.concatenate([d["b_route_group"][l], d["b_route_expert"][l].reshape(32)]))
    perms = [_perm(_stream_dil(s)) for s in range(NSTREAM)]
    ocg = np.empty((128, 128, 512), k3res[0]["oc"].dtype)
    for c in range(NC):
        ocg[c::8] = k3res[c]["oc"].reshape(16, 128, 512)
    ocg = ocg.reshape(NC * TL, 512)
    maps = []
    for c in range(NC):
        O = k3res[c]["Oab"]
        nat = np.empty_like(O)
        for s in range(NSTREAM):
            nat[s][perms[s]] = O[s]
        maps.append({
            "x": np.ascontiguousarray(xcur[c * TL:(c + 1) * TL]),
            "Oa": np.ascontiguousarray(nat[:4].transpose(1, 0, 2).reshape(TL, 260)),
            "Ob": np.ascontiguousarray(nat[4:].reshape(3, 4, TL, 65).transpose(0, 2, 1, 3).reshape(3, TL, 260)),
            "oc": np.ascontiguousarray(ocg[c * TL:(c + 1) * TL]), "esk": bc(d["sinks"][l]), "wout": d["w_out"][l], "lnp": lnp, "wr": wr, "br": br,
            "wg": d["w_expert_gate"][l], "wu": d["w_expert_up"][l], "wd": d["w_expert_down"][l],
        })
    return maps


_PROGS = {}


def _prog(name, fn):
    if name not in _PROGS:
        _PROGS[name] = fn()
    return _PROGS[name]


def kernel(**inputs):
    d = {k: np.asarray(v) for k, v in inputs.items()}
    x = np.ascontiguousarray(d["x"][0], dtype=np.float32)
    for l in range(4):
        r1 = _run(_prog("k1", build_k1), [{"xT": np.ascontiguousarray(x[c * TL:(c + 1) * TL].T), "w": d["w_in"][l]} for c in range(NC)])
        h = np.concatenate([r["h"] for r in r1], axis=0)
        r2 = _run(_prog("k2", build_k2), k2_inputs(h, d, l))
        r3 = _run(_prog("k3", build_k3), k3_inputs(h, r2, d))
        r4 = _run(_prog("k4", build_k4), k4_inputs(x, r3, d, l))
        x = np.concatenate([r["x2"] for r in r4], axis=0)
    return x[None].astype(np.float32)
```

```python
import contextlib
import math
import numpy as np
import concourse.bass as bass
import concourse.mybir as mybir
from concourse.bass_utils import run_bass_kernel_spmd

F32 = mybir.dt.float32
BF16 = mybir.dt.bfloat16
I32 = mybir.dt.int32
AF = mybir.ActivationFunctionType
ALU = mybir.AluOpType
AX = mybir.AxisListType

EPOCH = 30000
COMPUTE = ("act", "dve", "pool", "pe")


class _Op:
    __slots__ = ("eng", "fn", "deps", "dma", "key", "seq", "val")


class Sched:
    def __init__(self, nc, stack):
        self.nc = nc
        self.stack = stack
        self.ops = []
        self.lastw = {}
        self.readers = {}
        self.nseq = {e: 0 for e in ("sync", "act", "dve", "pool", "pe")}
        self.dmacount = {}
        self.n_tensors = 0

    def sbuf(self, name, shape, dtype):
        return self.stack.enter_context(self.nc.sbuf_tensor(name, list(shape), dtype))

    def psum(self, name, shape, dtype):
        return self.stack.enter_context(self.nc.psum_tensor(name, list(shape), dtype))

    def add(self, eng, fn, reads=(), writes=(), dma=False, key=None):
        op = _Op()
        op.eng, op.fn, op.dma = eng, fn, dma
        idx = len(self.ops)
        deps = {}
        for r in reads:
            w = self.lastw.get(r)
            if w is not None:
                deps[w] = "raw"
        for w_ in writes:
            w = self.lastw.get(w_)
            if w is not None and w not in deps:
                deps[w] = "waw"
            for rd in self.readers.get(w_, ()):
                if rd not in deps:
                    deps[rd] = "war"
        op.deps = deps
        if dma:
            if key is None:
                r0 = writes[0]
                key = r0[0] if isinstance(r0, tuple) else r0
            op.key = key
            c = self.dmacount.get(key, 0) + 1
            self.dmacount[key] = c
            op.val = 16 * c
        else:
            op.key = None
            self.nseq[eng] += 1
            op.seq = self.nseq[eng]
        for w_ in writes:
            self.lastw[w_] = idx
            self.readers[w_] = []
        for r in reads:
            self.readers.setdefault(r, []).append(idx)
        self.ops.append(op)
        return idx

    def emit(self):
        nc, stack = self.nc, self.stack
        csem = {}
        for e in COMPUTE:
            n_ep = self.nseq[e] // EPOCH + 1
            csem[e] = [stack.enter_context(nc.semaphore(f"c_{e}_{i}")) for i in range(n_ep)]
        dsem = {k: stack.enter_context(nc.semaphore(f"d_{i}")) for i, k in enumerate(self.dmacount)}
        per_eng = {e: [] for e in self.nseq}
        waited = {e: {} for e in self.nseq}
        dmaseen = {k: 0 for k in self.dmacount}
        plan = []
        for op in self.ops:
            need = {}
            for d, kind in op.deps.items():
                dop = self.ops[d]
                if dop.dma:
                    s = dsem[dop.key]
                    v = dmaseen[dop.key]
                    assert v >= dop.val
                else:
                    if dop.eng == op.eng and not op.dma:
                        if op.eng == "pe" or kind != "raw":
                            continue
                    ep = (dop.seq - 1) // EPOCH
                    s = csem[dop.eng][ep]
                    v = dop.seq - ep * EPOCH
                if need.get(s, (0,))[0] < v:
                    need[s] = (v, s)
            waits = []
            wd = waited[op.eng]
            for s, (v, _) in need.items():
                if wd.get(s, 0) >= v:
                    continue
                wd[s] = v
                waits.append((s, v))
            if op.dma:
                dmaseen[op.key] = op.val
                inc = (dsem[op.key], 16)
            else:
                ep = (op.seq - 1) // EPOCH
                inc = (csem[op.eng][ep], 1)
            per_eng[op.eng].append((op.fn, waits, inc))
        self.per_eng = per_eng
        final_waits = [(dsem[k], 16 * c) for k, c in self.dmacount.items()]

        def run(engname, eng):
            for fn, waits, inc in per_eng[engname]:
                for s, v in waits:
                    eng.wait_ge(s, v)
                ins = fn(eng)
                ins.then_inc(inc[0], inc[1])

        with nc.Block() as block:
            @block.sync
            def _(e):
                run("sync", e)
                for s, v in final_waits:
                    e.wait_ge(s, v)

            @block.scalar
            def _(e):
                run("act", e)

            @block.vector
            def _(e):
                run("dve", e)

            @block.gpsimd
            def _(e):
                run("pool", e)

            @block.tensor
            def _(e):
                run("pe", e)


NC = 8
TL = 2048
NT = 16
D = 1024
N_IN = 3232
NEG = -30000.0


def _run(nc, in_maps):
    res = run_bass_kernel_spmd(nc, in_maps, core_ids=list(range(NC)))
    return res.results


def build_k1():
    nc = bass.Bass("TRN2", target_bir_lowering=False)
    xT = nc.dram_tensor("xT", [D, TL], F32, kind="ExternalInput").ap()
    w = nc.dram_tensor("w", [D, N_IN], F32, kind="ExternalInput").ap()
    h = nc.dram_tensor("h", [TL, N_IN], BF16, kind="ExternalOutput").ap()
    with contextlib.ExitStack() as stack:
        S = Sched(nc, stack)
        xf = [S.sbuf(f"xf{i}", [128, TL], F32) for i in range(2)]
        xb = S.sbuf("xb", [128, 8, TL], BF16)
        for kc in range(8):
            b = kc % 2
            S.add("sync", lambda e, kc=kc, b=b: e.dma_start(out=xf[b][:], in_=xT[kc * 128:(kc + 1) * 128, :]),
                  writes=[("xf", b)], dma=True, key=f"xf{b}")
            S.add("dve" if b else "pool", lambda e, kc=kc, b=b: e.tensor_copy(out=xb[:, kc, :], in_=xf[b][:]),
                  reads=[("xf", b)], writes=[("xb", kc)])
        wf = [S.sbuf(f"wf{i}", [128, 8, 512], F32) for i in range(2)]
        wb = [S.sbuf(f"wb{i}", [128, 8, 512], BF16) for i in range(2)]
        ps = [S.psum(f"ps{i}", [128, 512], F32) for i in range(4)]
        hs = [S.sbuf(f"hs{i}", [128, 512], BF16) for i in range(4)]
        wv = w.rearrange("(c p) n -> p c n", p=128)
        for cb in range(7):
            ncol = 512 if cb < 6 else N_IN - 6 * 512
            c0 = cb * 512
            b = cb % 2
            S.add("sync", lambda e, b=b, c0=c0, ncol=ncol: e.dma_start(out=wf[b][:, :, :ncol], in_=wv[:, :, c0:c0 + ncol]),
                  writes=[("wf", b)], dma=True, key=f"wf{b}")
            S.add("pool", lambda e, b=b, ncol=ncol: e.tensor_copy(out=wb[b][:, :, :ncol], in_=wf[b][:, :, :ncol]),
                  reads=[("wf", b)], writes=[("wb", b)])
            for tt in range(NT):
                i = (cb * NT + tt) % 4
                for kc in range(8):
                    S.add("pe", lambda e, i=i, kc=kc, tt=tt, b=b, ncol=ncol: e.matmul(
                        ps[i][:, :ncol], lhsT=xb[:, kc, tt * 128:(tt + 1) * 128], rhs=wb[b][:, kc, :ncol],
                        start=(kc == 0), stop=(kc == 7)),
                        reads=[("xb", kc), ("wb", b)], writes=[("ps", i)])
                S.add("act" if i % 2 else "dve", lambda e, i=i, ncol=ncol: (e.copy if i % 2 else e.tensor_copy)(
                    out=hs[i][:, :ncol], in_=ps[i][:, :ncol]), reads=[("ps", i)], writes=[("hs", i)])
                S.add("pool", lambda e, i=i, tt=tt, c0=c0, ncol=ncol: e.dma_start(
                    out=h[tt * 128:(tt + 1) * 128, c0:c0 + ncol], in_=hs[i][:, :ncol]),
                    reads=[("hs", i)], writes=[("h", cb, tt)], dma=True, key=f"hso{i}")
        S.emit()
    return nc


def build_k2():
    nc = bass.Bass("TRN2", target_bir_lowering=False)
    di = lambda n, s, t: nc.dram_tensor(n, s, t, kind="ExternalInput").ap()
    cqT = di("cqT", [256, TL], BF16)
    ckvT = di("ckvT", [128, TL], BF16)
    kr2 = di("kr2", [2, 32, TL], BF16)
    gq = di("gq", [128, 2], F32)
    gkv = di("gkv", [128, 1], F32)
    wuq = di("wuq", [256, 768], F32)
    wuqs = di("wuqs", [256, 768], F32)
    wk = di("wk", [128, 512], F32)
    wv = di("wv", [128, 512], F32)
    cs2 = di("cs2", [2, 32, TL], F32)
    qT = nc.dram_tensor("qT", [8, 96, TL], BF16, kind="ExternalOutput").ap()
    KT = nc.dram_tensor("KT", [8, 96, TL], BF16, kind="ExternalOutput").ap()
    V = nc.dram_tensor("V", [TL, 512], BF16, kind="ExternalOutput").ap()
    with contextlib.ExitStack() as stack:
        S = Sched(nc, stack)
        cq = S.sbuf("cq", [128, 2, TL], BF16)
        ckv = S.sbuf("ckv", [128, TL], BF16)
        kr = S.sbuf("kr", [96, TL], BF16)
        krs = S.sbuf("krs", [96, TL], BF16)
        C2 = S.sbuf("C2", [96, TL], F32)
        S2 = S.sbuf("S2", [96, TL], F32)
        gq_s = S.sbuf("gq_s", [128, 2], F32)
        gkv_s = S.sbuf("gkv_s", [128, 1], F32)
        wf = S.sbuf("wf", [128, 2, 768], F32)
        wuq_b = S.sbuf("wuq_b", [128, 2, 768], BF16)
        wuqs_b = S.sbuf("wuqs_b", [128, 2, 768], BF16)
        wkf = S.sbuf("wkf", [128, 512], F32)
        wk_b = S.sbuf("wk_b", [128, 512], BF16)
        wv_b = S.sbuf("wv_b", [128, 512], BF16)
        ones = S.sbuf("ones", [128, 128], F32)
        sq = S.sbuf("sq", [128, TL], F32)
        rq = S.sbuf("rq", [128, TL], F32)
        rk = S.sbuf("rk", [128, TL], F32)
        cqn = S.sbuf("cqn", [128, 2, TL], BF16)
        ckvn = S.sbuf("ckvn", [128, TL], BF16)
        krr = S.sbuf("krr", [96, TL], BF16)
        t1 = S.sbuf("t1", [96, TL], F32)
        t2 = S.sbuf("t2", [96, TL], F32)
        ld = lambda dst, src, res, key: S.add("sync", lambda e: e.dma_start(out=dst, in_=src), writes=[res], dma=True, key=key)
        ld(cq[:], cqT.rearrange("(c p) n -> p c n", p=128), "cq", "cq")
        ld(ckv[:], ckvT, "ckv", "ckv")
        ld(kr[64:96, :], kr2[0], "kr", "kr")
        ld(krs[64:96, :], kr2[1], "krs", "krs")
        ld(C2[64:96, :], cs2[0], "C2", "C2")
        ld(S2[64:96, :], cs2[1], "S2", "S2")
        ld(gq_s[:], gq, "gq", "gq")
        ld(gkv_s[:], gkv, "gkv", "gkv")
        S.add("pool", lambda e: e.memset(ones[:], 1.0), writes=["ones"])
        epsc = S.sbuf("epsc", [128, 1], F32)
        S.add("pool", lambda e: e.memset(epsc[:], 1e-6), writes=["epsc"])
        ld(wf[:], wuq.rearrange("(c p) n -> p c n", p=128), "wf", "wf")
        S.add("dve", lambda e: e.tensor_copy(out=wuq_b[:], in_=wf[:]), reads=["wf"], writes=["wuq_b"])
        ld(wf[:], wuqs.rearrange("(c p) n -> p c n", p=128), "wf", "wf")
        S.add("dve", lambda e: e.tensor_copy(out=wuqs_b[:], in_=wf[:]), reads=["wf"], writes=["wuqs_b"])
        ld(wkf[:], wk, "wkf", "wkf")
        S.add("dve", lambda e: e.tensor_copy(out=wk_b[:], in_=wkf[:]), reads=["wkf"], writes=["wk_b"])
        ld(wkf[:], wv, "wkf", "wkf")
        S.add("dve", lambda e: e.tensor_copy(out=wv_b[:], in_=wkf[:]), reads=["wkf"], writes=["wv_b"])
        ps = [S.psum(f"ps{i}", [128, 512], F32) for i in range(6)]
        pi = [0]

        def nps():
            pi[0] = (pi[0] + 1) % 6
            return pi[0]

        def rstd(src_chunks, nfeat, eps, out_r, tag):
            nchunk = len(src_chunks)
            for g4 in range(4):
                cs = slice(g4 * 512, (g4 + 1) * 512)
                i = nps()
                for kc, (src, res) in enumerate(src_chunks):
                    S.add("dve", lambda e, src=src, cs=cs: e.tensor_tensor(out=sq[:, cs], in0=src[:, cs], in1=src[:, cs], op=ALU.mult),
                          reads=[res], writes=[("sq", g4)])
                    S.add("pe", lambda e, i=i, cs=cs, kc=kc: e.matmul(ps[i][:], lhsT=ones[:], rhs=sq[:, cs], start=(kc == 0), stop=(kc == nchunk - 1)),
                          reads=["ones", ("sq", g4)], writes=[("ps", i)])
                S.add("act", lambda e, i=i, cs=cs: e.activation(out=out_r[:, cs], in_=ps[i][:], func=AF.Sqrt, bias=epsc[:, 0:1], scale=1.0 / nfeat),
                      reads=[("ps", i), "epsc"], writes=[(tag, g4)])
                S.add("dve", lambda e, cs=cs: e.reciprocal(out=out_r[:, cs], in_=out_r[:, cs]),
                      reads=[(tag, g4)], writes=[(tag, g4)])

        rstd([(cq[:, 0, :], "cq"), (cq[:, 1, :], "cq")], 256, 1e-6, rq, "rq")
        for kc in range(2):
            for g4 in range(4):
                cs = slice(g4 * 512, (g4 + 1) * 512)
                S.add("dve", lambda e, kc=kc, cs=cs: e.scalar_tensor_tensor(out=cqn[:, kc, cs], in0=cq[:, kc, cs], scalar=gq_s[:, kc:kc + 1], in1=rq[:, cs], op0=ALU.mult, op1=ALU.mult),
                      reads=["cq", "gq", ("rq", g4)], writes=[("cqn", kc, g4)])
        rstd([(ckv, "ckv")], 128, 1e-6, rk, "rk")
        for g4 in range(4):
            cs = slice(g4 * 512, (g4 + 1) * 512)
            S.add("dve", lambda e, cs=cs: e.scalar_tensor_tensor(out=ckvn[:, cs], in0=ckv[:, cs], scalar=gkv_s[:, 0:1], in1=rk[:, cs], op0=ALU.mult, op1=ALU.mult),
                  reads=["ckv", "gkv", ("rk", g4)], writes=[("ckvn", g4)])
        S.add("dve", lambda e: e.tensor_tensor(out=t1[64:96, :], in0=kr[64:96, :], in1=C2[64:96, :], op=ALU.mult), reads=["kr", "C2"], writes=["t1"])
        S.add("pool", lambda e: e.tensor_tensor(out=t2[64:96, :], in0=krs[64:96, :], in1=S2[64:96, :], op=ALU.mult), reads=["krs", "S2"], writes=["t2"])
        S.add("dve", lambda e: e.tensor_tensor(out=krr[64:96, :], in0=t1[64:96, :], in1=t2[64:96, :], op=ALU.add), reads=["t1", "t2"], writes=["krr"])
        for h in range(8):
            S.add("pool", lambda e, h=h: e.dma_start(out=KT[h, 64:96, :], in_=krr[64:96, :]), reads=["krr"], writes=[("KTr", h)], dma=True, key="krro")
        qs = [S.sbuf(f"qs{i}", [96, 512], BF16) for i in range(2)]
        ks = [S.sbuf(f"ks{i}", [64, 512], BF16) for i in range(2)]
        u1 = [S.sbuf(f"u1{i}", [96, 512], F32) for i in range(2)]
        u2 = [S.sbuf(f"u2{i}", [96, 512], F32) for i in range(2)]
        n = 0
        for h in range(8):
            for g4 in range(4):
                cs = slice(g4 * 512, (g4 + 1) * 512)
                b = n % 2
                n += 1
                ia, ib, ik = nps(), nps(), nps()
                for kc in range(2):
                    S.add("pe", lambda e, ia=ia, kc=kc, h=h, cs=cs: e.matmul(ps[ia][0:96, :], lhsT=wuq_b[:, kc, h * 96:(h + 1) * 96], rhs=cqn[:, kc, cs], start=(kc == 0), stop=(kc == 1)),
                          reads=["wuq_b", ("cqn", kc, g4)], writes=[("ps", ia)])
                for kc in range(2):
                    S.add("pe", lambda e, ib=ib, kc=kc, h=h, cs=cs: e.matmul(ps[ib][0:96, :], lhsT=wuqs_b[:, kc, h * 96:(h + 1) * 96], rhs=cqn[:, kc, cs], start=(kc == 0), stop=(kc == 1)),
                          reads=["wuqs_b", ("cqn", kc, g4)], writes=[("ps", ib)])
                S.add("pe", lambda e, ik=ik, h=h, cs=cs: e.matmul(ps[ik][0:64, :], lhsT=wk_b[:, h * 64:(h + 1) * 64], rhs=ckvn[:, cs], start=True, stop=True),
                      reads=["wk_b", ("ckvn", g4)], writes=[("ps", ik)])
                S.add("act", lambda e, ia=ia, b=b: e.copy(out=qs[b][0:64, :], in_=ps[ia][0:64, :]), reads=[("ps", ia)], writes=[("qs", b, 0)])
                S.add("dve", lambda e, ia=ia, b=b, cs=cs: e.tensor_tensor(out=u1[b][64:96, :], in0=ps[ia][64:96, :], in1=C2[64:96, cs], op=ALU.mult),
                      reads=[("ps", ia), "C2"], writes=[("u1", b)])
                S.add("dve", lambda e, ib=ib, b=b, cs=cs: e.tensor_tensor(out=u2[b][64:96, :], in0=ps[ib][64:96, :], in1=S2[64:96, cs], op=ALU.mult),
                      reads=[("ps", ib), "S2"], writes=[("u2", b)])
                S.add("pool", lambda e, b=b: e.tensor_tensor(out=qs[b][64:96, :], in0=u1[b][64:96, :], in1=u2[b][64:96, :], op=ALU.add),
                      reads=[("u1", b), ("u2", b)], writes=[("qs", b, 1)])
                S.add("act", lambda e, ik=ik, b=b: e.copy(out=ks[b][:], in_=ps[ik][0:64, :]), reads=[("ps", ik)], writes=[("ks", b)])
                S.add("sync", lambda e, b=b, h=h, cs=cs: e.dma_start(out=qT[h, :, cs], in_=qs[b][:]), reads=[("qs", b, 0), ("qs", b, 1)], writes=[("qT", h, g4)], dma=True, key=f"qso{b}")
                S.add("sync", lambda e, b=b, h=h, cs=cs: e.dma_start(out=KT[h, 0:64, cs], in_=ks[b][:]), reads=[("ks", b)], writes=[("KTn", h, g4)], dma=True, key=f"kso{b}")
        vs = [S.sbuf(f"vs{i}", [128, 512], BF16) for i in range(2)]
        for tt in range(NT):
            b = tt % 2
            i = nps()
            S.add("pe", lambda e, i=i, tt=tt: e.matmul(ps[i][:], lhsT=ckvn[:, tt * 128:(tt + 1) * 128], rhs=wv_b[:], start=True, stop=True),
                  reads=["wv_b", ("ckvn", tt // 4)], writes=[("ps", i)])
            S.add("act", lambda e, i=i, b=b: e.copy(out=vs[b][:], in_=ps[i][:]), reads=[("ps", i)], writes=[("vs", b)])
            S.add("sync", lambda e, b=b, tt=tt: e.dma_start(out=V[tt * 128:(tt + 1) * 128, :], in_=vs[b][:]), reads=[("vs", b)], writes=[("V", tt)], dma=True, key=f"vso{b}")
        S.emit()
    return nc


def _rope_tables(c):
    inv = (10000.0 ** (-np.arange(0, 32, 2, dtype=np.float32) / 32)).astype(np.float32)
    pos = np.arange(c * TL, (c + 1) * TL, dtype=np.float32)
    ang = (pos[:, None] * inv[None, :]).astype(np.float32)
    cos, sin = np.cos(ang).astype(np.float32).T, np.sin(ang).astype(np.float32).T
    return np.ascontiguousarray(np.stack([np.concatenate([cos, cos], 0), np.concatenate([-sin, sin], 0)], 0))


def k2_inputs(h, d, l):
    wuq = d["w_uq"][l]
    wuqs = np.zeros_like(wuq)
    for hh in range(8):
        b = hh * 96 + 64
        wuqs[:, b:b + 16] = wuq[:, b + 16:b + 32]
        wuqs[:, b + 16:b + 32] = wuq[:, b:b + 16]
    wukv = d["w_ukv"][l].reshape(128, 8, 128)
    wk = np.ascontiguousarray(wukv[:, :, :64].reshape(128, 512))
    wv = np.ascontiguousarray(wukv[:, :, 64:].reshape(128, 512))
    gq = np.ascontiguousarray(d["mla_q_norm"][l].reshape(2, 128).T)
    gkv = np.ascontiguousarray(d["mla_kv_norm"][l].reshape(128, 1))
    maps = []
    for c in range(NC):
        hc = h[c * TL:(c + 1) * TL]
        krT = hc[:, 3200:3232].T
        maps.append({
            "cqT": np.ascontiguousarray(hc[:, 2816:3072].T),
            "ckvT": np.ascontiguousarray(hc[:, 3072:3200].T),
            "kr2": np.ascontiguousarray(np.stack([krT, np.concatenate([krT[16:], krT[:16]], 0)], 0)),
            "gq": gq, "gkv": gkv, "wuq": wuq, "wuqs": wuqs, "wk": wk, "wv": wv,
            "cs2": _rope_tables(c),
        })
    return maps


SCALE_C = 96 ** -0.5
NSTREAM = 16


def _stream_dil(s):
    return 1 if s < 4 else (1, 4, 16)[(s - 4) // 4]


def build_k3():
    nc = bass.Bass("TRN2", target_bir_lowering=False)
    di = lambda n, s, t: nc.dram_tensor(n, s, t, kind="ExternalInput").ap()
    qT = di("qT", [8, 96, TL], BF16)
    KTa = di("KTa", [8, 96, 8 * TL], BF16)
    Va = di("Va", [8, 128, 128, 64], BF16)
    mkc = di("mkc", [128, 32, 512], BF16)
    QTb = di("QTb", [NSTREAM, 64, TL], BF16)
    KTb = di("KTb", [NSTREAM, 64, 16, 256], BF16)
    Vb = di("Vb", [NSTREAM, 128, 32, 64], BF16)
    bT = di("bT", [128, NSTREAM, 2, 256], F32)
    oc = nc.dram_tensor("oc", [TL, 512], BF16, kind="ExternalOutput").ap()
    Oab = nc.dram_tensor("Oab", [NSTREAM, TL, 65], F32, kind="ExternalOutput").ap()
    with contextlib.ExitStack() as stack:
        S = Sched(nc, stack)
        mk_s = S.sbuf("mk_s", [128, 32, 512], BF16)
        bT_s = S.sbuf("bT_s", [128, NSTREAM, 2, 256], F32)
        ld = lambda eng, dst, src, res, key: S.add(eng, lambda e: e.dma_start(out=dst, in_=src), writes=[res], dma=True, key=key)
        ld("sync", mk_s[:], mkc, "mk", "mk")
        ld("sync", bT_s[:], bT, "bT", "bT")
        Sps = [S.psum(f"Sps{i}", [128, 512], F32) for i in range(4)]
        Ops = [S.psum(f"Ops{i}", [128, 512], F32) for i in range(2)]
        Bps = [S.psum(f"Bps{i}", [128, 4, 65], F32) for i in range(2)]
        Pt = [S.sbuf(f"Pt{i}", [128, 512], BF16) for i in range(4)]
        tmp = [S.sbuf(f"tmp{i}", [128, 512], F32) for i in range(2)]
        qb = [S.sbuf(f"qb{i}", [64, TL], BF16) for i in range(2)]
        kb = [S.sbuf(f"kb{i}", [64, 16, 256], BF16) for i in range(2)]
        vb = [S.sbuf(f"vb{i}", [128, 32, 65], BF16) for i in range(2)]
        obs = [S.sbuf(f"obs{i}", [128, 16, 65], F32) for i in range(2)]
        for i in range(2):
            S.add("pool", lambda e, i=i: e.memset(vb[i][:], 1.0), writes=[("vb", i)])
        LA = 2
        items = []
        cnt = 0
        for s in range(NSTREAM):
            sb_ = s % 2
            d = _stream_dil(s)
            per = 16 // d
            for pr in range(8):
                si = cnt % 4
                ti = cnt % 2
                bi = cnt % 2
                cnt += 1

                def front(s=s, sb_=sb_, per=per, pr=pr, si=si, ti=ti):
                    if pr == 0:
                        ld("sync", qb[sb_][:], QTb[s], ("qb", sb_), f"qb{sb_}")
                        ld("sync", kb[sb_][:], KTb[s], ("kb", sb_), f"kb{sb_}")
                        S.add("sync", lambda e: e.dma_start(out=vb[sb_][:, :, 0:64], in_=Vb[s]), reads=[("vb", sb_)], writes=[("vb", sb_)], dma=True, key=f"vb{sb_}")
                    for j in range(2):
                        b = pr * 2 + j
                        for half in range(2):
                            S.add("pe", lambda e, j=j, half=half, b=b: e.matmul(
                                Sps[si][:, j * 256 + half * 128: j * 256 + half * 128 + 128], lhsT=kb[sb_][:, b, half * 128:(half + 1) * 128],
                                rhs=qb[sb_][:, b * 128:(b + 1) * 128], start=True, stop=True),
                                reads=[("kb", sb_), ("qb", sb_)], writes=[("S", si)])
                    for j in range(2):
                        b = pr * 2 + j
                        var = 0 if (b % per) == 0 else 1
                        S.add("dve", lambda e, j=j, var=var: e.scalar_tensor_tensor(
                            out=tmp[ti][:, j * 256:(j + 1) * 256], in0=Sps[si][:, j * 256:(j + 1) * 256], scalar=0.125,
                            in1=bT_s[:, s, var, :], op0=ALU.mult, op1=ALU.add),
                            reads=[("S", si), "bT"], writes=[("tmp", ti, j)])
                    S.add("act", lambda e: e.activation(out=Pt[si][:], in_=tmp[ti][:], func=AF.Exp),
                          reads=[("tmp", ti, 0), ("tmp", ti, 1)], writes=[("P", si)])

                def back(s=s, sb_=sb_, pr=pr, si=si, bi=bi):
                    for j in range(2):
                        b = pr * 2 + j
                        for half in range(2):
                            S.add("pe", lambda e, j=j, half=half, b=b: e.matmul(
                                Bps[bi][:, j, :], lhsT=Pt[si][:, j * 256 + half * 128: j * 256 + half * 128 + 128],
                                rhs=vb[sb_][:, b * 2 + half, :], start=(half == 0), stop=(half == 1)),
                                reads=[("P", si), ("vb", sb_)], writes=[("Bps", bi)])
                    S.add("dve", lambda e: e.tensor_copy(out=obs[sb_][:, pr * 2:pr * 2 + 2, :], in_=Bps[bi][:, 0:2, :]),
                          reads=[("Bps", bi)], writes=[("obs", sb_)])
                    if pr == 7:
                        S.add("pool", lambda e: e.dma_start(out=Oab[s].rearrange("(b p) d -> p b d", p=128), in_=obs[sb_][:]),
                              reads=[("obs", sb_)], writes=[("Oab", s)], dma=True, key=f"obso{sb_}")

                items.append((front, back))
        kall = S.sbuf("kall", [96, 8 * TL], BF16)
        vall = S.sbuf("vall", [128, 128, 65], BF16)
        qh = [S.sbuf(f"qh{i}", [96, TL], BF16) for i in range(2)]
        OsT = [S.sbuf(f"OsT{i}", [65, 512], F32) for i in range(2)]
        Osb = [S.sbuf(f"Osb{i}", [128, 4, 65], F32) for i in range(2)]
        rc = [S.sbuf(f"rc{i}", [128, 4, 1], F32) for i in range(2)]
        ocs = S.sbuf("ocs", [128, NT, 512], BF16)
        identf = S.sbuf("identf", [128, 128], F32)
        S.add("pool", lambda e: e.memset(identf[:], 1.0), writes=["identf"])
        S.add("pool", lambda e: e.affine_select(out=identf[:], in_=identf[:], pattern=[[-1, 128]], compare_op=ALU.is_equal, fill=0.0, base=0, channel_multiplier=1),
              reads=["identf"], writes=["identf"])
        S.add("pool", lambda e: e.memset(vall[:], 1.0), writes=[("vq", q) for q in range(4)])

        def load_quarter(h, q):
            ks = slice(q * 4096, (q + 1) * 4096)
            S.add("sync", lambda e: e.dma_start(out=kall[:, ks], in_=KTa[h, :, ks]), writes=[("kq", q)], dma=True, key=f"kq{q}")
            S.add("sync", lambda e: e.dma_start(out=vall[:, q * 32:(q + 1) * 32, 0:64], in_=Va[h, :, q * 32:(q + 1) * 32, :]), reads=[("vq", q)], writes=[("vq", q)], dma=True, key=f"vq{q}")

        ocnt = 0
        item_groups = [items]
        for h in range(8):
            hb = h % 2
            items = []
            item_groups.append(items)
            for g in (3, 2, 1, 0):
                ob = ocnt % 2
                ocnt += 1
                ntile = 32 * (g + 1)
                last = ntile - 1
                qs_ = slice(g * 512, (g + 1) * 512)
                for idx in range(ntile):
                    si = cnt % 4
                    ti = cnt % 2
                    cnt += 1
                    z = idx - 32 * g

                    def front(h=h, hb=hb, g=g, idx=idx, z=z, si=si, ti=ti, qs_=qs_):
                        if g == 3 and idx == 0:
                            ld("sync", qh[hb][:], qT[h], ("qh", hb), f"qh{hb}")
                            if h == 0:
                                for q in range(4):
                                    load_quarter(0, q)
                            else:
                                load_quarter(h, 0)
                        if idx == 3 and g < 3 and h < 7:
                            load_quarter(h + 1, g + 1)
                        S.add("pe", lambda e: e.matmul(Sps[si][:], lhsT=kall[:, idx * 128:(idx + 1) * 128], rhs=qh[hb][:, qs_], start=True, stop=True),
                              reads=[("kq", idx // 32), ("qh", hb)], writes=[("S", si)])
                        if z < 0:
                            S.add("act", lambda e: e.activation(out=Pt[si][:], in_=Sps[si][:], func=AF.Exp, scale=SCALE_C),
                                  reads=[("S", si)], writes=[("P", si)])
                        else:
                            S.add("dve", lambda e: e.scalar_tensor_tensor(out=tmp[ti][:], in0=Sps[si][:], scalar=SCALE_C, in1=mk_s[:, z, :], op0=ALU.mult, op1=ALU.add),
                                  reads=[("S", si), "mk"], writes=[("tmp", ti, 0), ("tmp", ti, 1)])
                            S.add("act", lambda e: e.activation(out=Pt[si][:], in_=tmp[ti][:], func=AF.Exp),
                                  reads=[("tmp", ti, 0), ("tmp", ti, 1)], writes=[("P", si)])

                    def back(h=h, g=g, idx=idx, last=last, si=si, ob=ob):
                        S.add("pe", lambda e: e.matmul(Ops[ob][0:65, :], lhsT=vall[:, idx, :], rhs=Pt[si][:], start=(idx == 0), stop=(idx == last)),
                              reads=[("P", si), ("vq", idx // 32)], writes=[("Ops", ob)])
                        if idx == last:
                            S.add("act", lambda e: e.copy(out=OsT[ob][:], in_=Ops[ob][0:65, :]), reads=[("Ops", ob)], writes=[("OsT", ob)])
                            for sb in range(4):
                                S.add("pe", lambda e, sb=sb: e.matmul(Bps[ob][:, sb, :], lhsT=OsT[ob][:, sb * 128:(sb + 1) * 128], rhs=identf[0:65, 0:65], start=True, stop=True),
                                      reads=[("OsT", ob), "identf"], writes=[("Bps", ob)])
                            S.add("dve", lambda e: e.tensor_copy(out=Osb[ob][:], in_=Bps[ob][:]), reads=[("Bps", ob)], writes=[("Osb", ob)])
                            S.add("dve", lambda e: e.reciprocal(out=rc[ob][:], in_=Osb[ob][:, :, 64:65]), reads=[("Osb", ob)], writes=[("rc", ob)])
                            for sb in range(4):
                                S.add("pool", lambda e, sb=sb: e.tensor_scalar(
                                    out=ocs[:, g * 4 + sb, h * 64:(h + 1) * 64], in0=Osb[ob][:, sb, 0:64], scalar1=rc[ob][:, sb, 0:1], scalar2=None, op0=ALU.mult),
                                    reads=[("Osb", ob), ("rc", ob)], writes=[("ocs", h, g)])

                    items.append((front, back))
        for items in item_groups:
            for k in range(len(items) + LA):
                if k < len(items):
                    items[k][0]()
                if k >= LA:
                    items[k - LA][1]()
        S.add("pool", lambda e: e.dma_start(out=oc.rearrange("(t p) d -> p t d", p=128), in_=ocs[:]),
              reads=[("ocs", h, qt) for h in range(8) for qt in range(4)], writes=["oc"], dma=True, key="oco")
        S.emit()
    return nc


def _perm(d):
    n = TL // d
    u = np.arange(TL)
    return (u % n) * d + u // n


def _t5_bucket(dist):
    df = np.maximum(dist, 1).astype(np.float32) / 16
    large = 16 + (np.log(df) / math.log(2048 / 16) * 16).astype(np.int32)
    return np.where(dist < 16, dist, np.minimum(large, 31))


def _stream_cols(s):
    if s < 4:
        return s * 64, 256 + (s // 2) * 64, 384 + (s // 2) * 64
    j = s - 4
    return 512 + j * 64, 1280 + j * 64, 2048 + j * 64


def _bias_tables(rel_bias, c):
    kj = np.arange(128)[:, None]
    qi = np.arange(128)[None, :]
    out = np.empty((128, NSTREAM, 2, 256), np.float32)
    for s in range(NSTREAM):
        d = _stream_dil(s)
        maxd = 127 if s < 4 else 128
        col = rel_bias[:, s]
        dp = 128 + qi - kj
        dc = qi - kj
        tp = np.where(dp <= maxd, col[_t5_bucket(dp * d)], np.float32(NEG)).astype(np.float32)
        tc = np.where(dc >= 0, col[_t5_bucket(np.maximum(dc, 0) * d)], np.float32(NEG)).astype(np.float32)
        out[:, s, 0, :128] = tp if c > 0 else np.float32(NEG)
        out[:, s, 1, :128] = tp
        out[:, s, 0, 128:] = tc
        out[:, s, 1, 128:] = tc
    return out


def k3_inputs(h, k2res, d):
    KTa = np.ascontiguousarray(np.concatenate([r["KT"] for r in k2res], axis=2))
    Va = np.ascontiguousarray(np.concatenate([r["V"] for r in k2res], axis=0).reshape(128, 128, 8, 64).transpose(2, 1, 0, 3))
    qTa = np.concatenate([r["qT"] for r in k2res], axis=2).reshape(8, 96, 128, 128)
    p_ = np.arange(128)[:, None, None, None]
    z_ = np.arange(32)[None, :, None, None]
    j_ = np.arange(4)[None, None, :, None]
    i_ = np.arange(128)[None, None, None, :]
    Kd, Vd, Qd = {}, {}, {}
    for s in range(NSTREAM):
        qc, kc, vc = _stream_cols(s)
        perm = _perm(_stream_dil(s))
        for c in range(NC):
            rows = c * TL + perm
            Qd[s, c] = h[rows, qc:qc + 64]
            Kd[s, c] = h[rows, kc:kc + 64].reshape(16, 128, 64)
            Vd[s, c] = h[rows, vc:vc + 64].reshape(16, 128, 64)
    maps = []
    for c in range(NC):
        QTb = np.empty((NSTREAM, 64, TL), h.dtype)
        KTb = np.empty((NSTREAM, 64, 16, 256), h.dtype)
        Vb = np.empty((NSTREAM, 128, 32, 64), h.dtype)
        for s in range(NSTREAM):
            per = 16 // _stream_dil(s)
            QTb[s] = Qd[s, c].T
            for b in range(16):
                if b % per:
                    kprev, vprev = Kd[s, c][b - 1], Vd[s, c][b - 1]
                elif c > 0:
                    bb = (b // per) * per + per - 1
                    kprev, vprev = Kd[s, c - 1][bb], Vd[s, c - 1][bb]
                else:
                    kprev, vprev = np.zeros((128, 64), h.dtype), np.zeros((128, 64), h.dtype)
                KTb[s, :, b, :128] = kprev.T
                KTb[s, :, b, 128:] = Kd[s, c][b].T
                Vb[s, :, 2 * b] = vprev
                Vb[s, :, 2 * b + 1] = Vd[s, c][b]
        vis_ = (z_ < c + 8 * j_) | ((z_ == c + 8 * j_) & (p_ <= i_))
        mkc = np.where(vis_, np.float32(0), np.float32(NEG)).astype(h.dtype).reshape(128, 32, 512)
        maps.append({
            "qT": np.ascontiguousarray(qTa[:, :, c::8, :].reshape(8, 96, TL)), "KTa": KTa, "Va": Va, "mkc": mkc,
            "QTb": QTb, "KTb": KTb, "Vb": Vb, "bT": _bias_tables(d["rel_bias"], c),
        })
    return maps


ALPHA = 8 ** 0.25
NE = 32
DEBUG = False
CAP = 256


def build_k4():
    nc = bass.Bass("TRN2", target_bir_lowering=False)
    di = lambda n, s, t: nc.dram_tensor(n, s, t, kind="ExternalInput").ap()
    x = di("x", [TL, D], F32)
    Oa = di("Oa", [TL, 260], F32)
    Ob = di("Ob", [3, TL, 260], F32)
    oc = di("oc", [TL, 512], BF16)
    esk = di("esk", [128, 4], F32)
    wout = di("wout", [D, D], F32)
    lnp = di("lnp", [4, 128, D], F32)
    wr = di("wr", [D, 36], F32)
    br = di("br", [128, 36], F32)
    wgu = di("wgu", [NE, 128, 8, 512], F32)
    wd = di("wd", [NE, 128, 2, D], F32)
    eoff = di("eoff", [128, NT * NE], F32)
    Xg = nc.dram_tensor("Xg", [NE * CAP, D], BF16).ap()
    Yg = nc.dram_tensor("Yg", [NE * CAP, D], F32).ap()
    x2 = nc.dram_tensor("x2", [TL, D], F32, kind="ExternalOutput").ap()
    dbg = nc.dram_tensor("dbg", [128, NT * 2], I32, kind="ExternalOutput").ap() if DEBUG else None
    dbg2 = nc.dram_tensor("dbg2", [128, NT * 2], F32, kind="ExternalOutput").ap() if DEBUG else None
    with contextlib.ExitStack() as stack:
        S = Sched(nc, stack)
        ld = lambda eng, dst, src, res, key: S.add(eng, lambda e: e.dma_start(out=dst, in_=src), writes=[res], dma=True, key=key)
        xs = S.sbuf("xs", [128, NT, D], F32)
        x1b = S.sbuf("x1b", [128, NT, D], BF16)
        oh1 = S.sbuf("oh1", [128, NT, NE], F32)
        oh2 = S.sbuf("oh2", [128, NT, NE], F32)
        g12 = S.sbuf("g12", [128, NT, 2], F32)
        zrow = S.sbuf("zrow", [128, D], BF16)
        lng = S.sbuf("lng", [128, D], F32)
        lnb = S.sbuf("lnb", [128, D], F32)
        esk_s = S.sbuf("esk_s", [128, 4], F32)
        ident_f = S.sbuf("ident_f", [128, 128], F32)
        ident = S.sbuf("ident", [128, 128], BF16)
        epsc = S.sbuf("epsc", [128, 1], F32)
        wsf = S.sbuf("wsf", [128, 8, 512], F32)
        wdf = S.sbuf("wdf", [128, 2, D], F32)
        wout_b = S.sbuf("wout_b", [128, 8, D], BF16)
        wgu_b = [wout_b[:, :, 0:512], wout_b[:, :, 512:1024]]
        wd_b = [S.sbuf(f"wd_b{i}", [128, 2, D], BF16) for i in range(2)]
        Tps = [S.psum(f"Tps{i}", [128, 8, 128], BF16) for i in range(2)]
        Gps = [S.psum(f"Gps{i}", [128, 512], F32) for i in range(2)]
        Yps = [S.psum("Yps0", [128, D], F32)] * 2
        T2ps = [S.psum(f"T2ps{i}", [128, 2, 128], BF16) for i in range(2)]
        S.add("pool", lambda e: e.memset(ident_f[:], 1.0), writes=["ident_f"])
        S.add("pool", lambda e: e.affine_select(out=ident_f[:], in_=ident_f[:], pattern=[[-1, 128]], compare_op=ALU.is_equal, fill=0.0, base=0, channel_multiplier=1),
              reads=["ident_f"], writes=["ident_f"])
        S.add("dve", lambda e: e.tensor_copy(out=ident[:], in_=ident_f[:]), reads=["ident_f"], writes=["ident"])
        S.add("pool", lambda e: e.memset(epsc[:], 1e-5), writes=["epsc"])
        ld("sync", esk_s[:], esk, "esk", "esk")
        S.add("act", lambda e: e.activation(out=esk_s[:], in_=esk_s[:], func=AF.Exp), reads=["esk"], writes=["esk"])
        ld("sync", lng[:], lnp[0], "lng", "lng")
        ld("sync", lnb[:], lnp[1], "lnb", "lnb")
        woutv = wout.rearrange("(c p) n -> p c n", p=128)
        for half in range(2):
            S.add("sync", lambda e, half=half: e.dma_start(out=wsf[:], in_=woutv[:, :, half * 512:(half + 1) * 512]), writes=[("wsfc", c) for c in range(8)], dma=True, key="wsf")
            S.add("dve", lambda e, half=half: e.tensor_copy(out=wout_b[:, :, half * 512:(half + 1) * 512], in_=wsf[:]), reads=[("wsfc", c) for c in range(8)], writes=[("wbigc", half, c) for c in range(8)])
        for tt in range(NT):
            ld("sync", xs[:, tt, :], x[tt * 128:(tt + 1) * 128, :], ("xs", tt), "xs")
        junk = S.sbuf("junk", [128, D], F32)
        sm = [S.sbuf(f"sm{i}", [128, 64], F32) for i in range(2)]
        p1 = contextlib.ExitStack()
        sb1 = lambda name, shape, dt: p1.enter_context(nc.sbuf_tensor(name, list(shape), dt))
        x1Tt = [sb1(f"x1Tt{i}", [128, 8, 128], BF16) for i in range(2)]
        br_s = sb1("br_s", [128, 36], F32)
        wrf = sb1("wrf", [128, 8, 36], F32)
        wr_b = sb1("wr_b", [128, 8, 36], BF16)
        ld("sync", br_s[:], br, "br", "br")
        ld("sync", wrf[:], wr.rearrange("(c p) n -> p c n", p=128), "wrf", "wrf")
        S.add("dve", lambda e: e.tensor_copy(out=wr_b[:], in_=wrf[:]), reads=["wrf"], writes=["wr_b"])
        oas = [sb1(f"oas{i}", [128, 4, 65], F32) for i in range(2)]
        obs = [sb1(f"obs{i}", [128, 3, 260], F32) for i in range(2)]
        nbs = [sb1(f"nbs{i}", [128, 4, 65], F32) for i in range(2)]
        obf = [sb1(f"obf{i}", [128, D], BF16) for i in range(2)]
        oT = [sb1(f"oT{i}", [128, 8, 128], BF16) for i in range(2)]
        rt = [sb1(f"rt{i}", [128, 256], F32) for i in range(2)]
        S.add("pool", lambda e: e.memset(zrow[:], 0.0), writes=["zrow"])
        for i in range(NE * CAP // 128):
            S.add("sync", lambda e, i=i: e.dma_start(out=Xg[i * 128:(i + 1) * 128, :], in_=zrow[:]), reads=["zrow"], writes=[("Xgz", i)], dma=True, key="xgz")

        def layer_norm(tt, b, gres, bres):
            X = xs[:, tt, :]
            R = ("xs", tt)
            m = sm[b]
            S.add("dve", lambda e: e.reduce_sum(out=m[:, 0:1], in_=X, axis=AX.X), reads=[R], writes=[("sm", b, 0)])
            S.add("dve", lambda e: e.tensor_scalar(out=m[:, 1:2], in0=m[:, 0:1], scalar1=-1.0 / D, scalar2=None, op0=ALU.mult), reads=[("sm", b, 0)], writes=[("sm", b, 1)])
            S.add("act", lambda e: e.activation(out=X, in_=X, func=AF.Identity, bias=m[:, 1:2]), reads=[R, ("sm", b, 1)], writes=[R])
            S.add("dve", lambda e: e.memset(m[:, 2:3], 0.0), writes=[("sm", b, 2)])
            S.add("act", lambda e: e.activation(out=junk[:], in_=X, func=AF.Square, accum_out=m[:, 2:3]), reads=[R, ("sm", b, 2)], writes=["junk", ("sm", b, 2)])
            S.add("act", lambda e: e.activation(out=m[:, 3:4], in_=m[:, 2:3], func=AF.Sqrt, bias=epsc[:, 0:1], scale=1.0 / D), reads=[("sm", b, 2), "epsc"], writes=[("sm", b, 3)])
            S.add("dve", lambda e: e.reciprocal(out=m[:, 4:5], in_=m[:, 3:4]), reads=[("sm", b, 3)], writes=[("sm", b, 4)])
            S.add("dve", lambda e: e.scalar_tensor_tensor(out=X, in0=X, scalar=m[:, 4:5], in1=lng[:], op0=ALU.mult, op1=ALU.mult), reads=[R, ("sm", b, 4), gres], writes=[R])
            S.add("dve", lambda e: e.tensor_tensor(out=X, in0=X, in1=lnb[:], op=ALU.add), reads=[R, bres], writes=[R])

        Oav = Oa.rearrange("t (h d) -> t h d", d=65)
        for tt in range(NT):
            b = tt % 2
            ts_ = slice(tt * 128, (tt + 1) * 128)
            ld("sync", oas[b][:], Oav[ts_], ("oas", b), f"oas{b}")
            S.add("sync", lambda e, b=b, ts_=ts_: e.dma_start(out=obs[b][:], in_=Ob[:, ts_, :].rearrange("g t f -> t g f")), writes=[("obs", b)], dma=True, key=f"obs{b}")
            S.add("sync", lambda e, b=b, ts_=ts_: e.dma_start(out=obf[b][:, 512:1024], in_=oc[ts_, :]), writes=[("obf", b, 2)], dma=True, key=f"obfc{b}")
            m = sm[b]
            S.add("dve", lambda e, b=b, m=m: e.tensor_tensor(out=m[:, 8:12], in0=oas[b][:, :, 64], in1=esk_s[:], op=ALU.add), reads=[("oas", b), "esk"], writes=[("sm", b, 8)])
            S.add("dve", lambda e, m=m: e.reciprocal(out=m[:, 12:16], in_=m[:, 8:12]), reads=[("sm", b, 8)], writes=[("sm", b, 12)])
            for hh in range(4):
                S.add("act", lambda e, b=b, m=m, hh=hh: e.activation(out=obf[b][:, hh * 64:(hh + 1) * 64], in_=oas[b][:, hh, 0:64], func=AF.Identity, scale=m[:, 12 + hh:13 + hh]),
                      reads=[("oas", b), ("sm", b, 12)], writes=[("obf", b, 0)])
            nbf = nbs[b][:].rearrange("p a b -> p (a b)")
            S.add("dve", lambda e, b=b, nbf=nbf: e.tensor_tensor(out=nbf, in0=obs[b][:, 0, :], in1=obs[b][:, 1, :], op=ALU.add), reads=[("obs", b)], writes=[("nbs", b)])
            S.add("dve", lambda e, b=b, nbf=nbf: e.tensor_tensor(out=nbf, in0=nbf, in1=obs[b][:, 2, :], op=ALU.add), reads=[("obs", b), ("nbs", b)], writes=[("nbs", b)])
            S.add("dve", lambda e, b=b, m=m: e.reciprocal(out=m[:, 16:20], in_=nbs[b][:, :, 64]), reads=[("nbs", b)], writes=[("sm", b, 16)])
            for hh in range(4):
                S.add("dve", lambda e, b=b, m=m, hh=hh: e.tensor_scalar(out=obf[b][:, 256 + hh * 64:256 + (hh + 1) * 64], in0=nbs[b][:, hh, 0:64], scalar1=m[:, 16 + hh:17 + hh], scalar2=None, op0=ALU.mult),
                      reads=[("nbs", b), ("sm", b, 16)], writes=[("obf", b, 1)])
            for kc in range(8):
                S.add("pe", lambda e, b=b, kc=kc: e.transpose(out=Tps[b][:, kc, :], in_=obf[b][:, kc * 128:(kc + 1) * 128], identity=ident[:]),
                      reads=[("obf", b, 0), ("obf", b, 1), ("obf", b, 2), "ident"], writes=[("Tps", b)])
            S.add("act", lambda e, b=b: e.copy(out=oT[b][:], in_=Tps[b][:]), reads=[("Tps", b)], writes=[("oT", b)])
            for half in range(2):
                for kc in range(8):
                    S.add("pe", lambda e, b=b, half=half, kc=kc: e.matmul(Yps[b][:, half * 512:(half + 1) * 512], lhsT=oT[b][:, kc, :], rhs=wout_b[:, kc, half * 512:(half + 1) * 512], start=(kc == 0), stop=(kc == 7)),
                          reads=[("oT", b), ("wbigc", half, kc)], writes=[("Yps", 0)])
            S.add("dve", lambda e, b=b, tt=tt: e.scalar_tensor_tensor(out=xs[:, tt, :], in0=xs[:, tt, :], scalar=ALPHA, in1=Yps[b][:], op0=ALU.mult, op1=ALU.add),
                  reads=[("xs", tt), ("Yps", 0)], writes=[("xs", tt)])
            layer_norm(tt, b, "lng", "lnb")
            S.add("act", lambda e, tt=tt: e.copy(out=x1b[:, tt, :], in_=xs[:, tt, :]), reads=[("xs", tt)], writes=[("x1b", tt)])
            for kc in range(8):
                S.add("pe", lambda e, b=b, kc=kc, tt=tt: e.transpose(out=Tps[b][:, kc, :], in_=x1b[:, tt, kc * 128:(kc + 1) * 128], identity=ident[:]),
                      reads=[("x1b", tt), "ident"], writes=[("Tps", b)])
            S.add("act", lambda e, b=b: e.copy(out=x1Tt[b][:], in_=Tps[b][:]), reads=[("Tps", b)], writes=[("x1Tt", b)])
            S.add("act", lambda e, tt=tt: e.mul(out=xs[:, tt, :], in_=xs[:, tt, :], mul=ALPHA), reads=[("xs", tt)], writes=[("xs", tt)])
            for kc in range(8):
                S.add("pe", lambda e, b=b, kc=kc, ts_=ts_: e.matmul(Gps[b][:, 0:36], lhsT=x1Tt[b][:, kc, :], rhs=wr_b[:, kc, :], start=(kc == 0), stop=(kc == 7)),
                      reads=[("x1Tt", b), "wr_b"], writes=[("Gps", b)])
            r = rt[b]
            L, E1, E2, q1, q2 = r[:, 0:36], r[:, 40:72], r[:, 72:104], r[:, 104:136], r[:, 136:168]
            RT = ("rt", b)
            dv = lambda fn, rd=(), wr_=(): S.add("dve", fn, reads=[RT, ("sm", b, 30)] + list(rd), writes=[RT, ("sm", b, 30)] + list(wr_))
            dv(lambda e, b=b, L=L: e.tensor_tensor(out=L, in0=Gps[b][:, 0:36], in1=br_s[:], op=ALU.add), rd=[("Gps", b), "br"])
            dv(lambda e, m=m, L=L: e.reduce_max(out=m[:, 30:31], in_=L[:, 0:4], axis=AX.X))
            dv(lambda e, m=m: e.tensor_scalar(out=m[:, 31:32], in0=m[:, 30:31], scalar1=-1.0, scalar2=None, op0=ALU.mult))
            S.add("dve", lambda e, m=m: e.memset(m[:, 36:37], 0.0), reads=[RT, ("sm", b, 30)], writes=[RT, ("sm", b, 30)])
            S.add("act", lambda e, m=m, L=L: e.activation(out=m[:, 32:36], in_=L[:, 0:4], func=AF.Exp, bias=m[:, 31:32], accum_out=m[:, 36:37]),
                  reads=[RT, ("sm", b, 30)], writes=[RT, ("sm", b, 30)])
            dv(lambda e, m=m: e.reciprocal(out=m[:, 37:38], in_=m[:, 36:37]))
            dv(lambda e, m=m, L=L: e.tensor_scalar(out=m[:, 40:44], in0=L[:, 0:4], scalar1=m[:, 30:31], scalar2=None, op0=ALU.is_equal))
            dv(lambda e, m=m: e.tensor_scalar(out=m[:, 44:48], in0=m[:, 40:44], scalar1=1e9, scalar2=-1e9, op0=ALU.mult, op1=ALU.add))
            for g in range(4):
                dv(lambda e, m=m, L=L, E1=E1, g=g: e.tensor_scalar(out=E1[:, 8 * g:8 * g + 8], in0=L[:, 4 + 8 * g:12 + 8 * g], scalar1=m[:, 44 + g:45 + g], scalar2=None, op0=ALU.add))
            dv(lambda e, m=m, E1=E1: e.reduce_max(out=m[:, 48:49], in_=E1, axis=AX.X))
            dv(lambda e, m=m, E1=E1, q1=q1: e.tensor_scalar(out=q1, in0=E1, scalar1=m[:, 48:49], scalar2=None, op0=ALU.is_equal))
            dv(lambda e, E1=E1, E2=E2, q1=q1: e.scalar_tensor_tensor(out=E2, in0=q1, scalar=-1e9, in1=E1, op0=ALU.mult, op1=ALU.add))
            dv(lambda e, m=m, E2=E2: e.reduce_max(out=m[:, 49:50], in_=E2, axis=AX.X))
            dv(lambda e, m=m, E2=E2, q2=q2: e.tensor_scalar(out=q2, in0=E2, scalar1=m[:, 49:50], scalar2=None, op0=ALU.is_equal))
            dv(lambda e, m=m: e.tensor_tensor(out=m[:, 50:51], in0=m[:, 49:50], in1=m[:, 48:49], op=ALU.subtract))
            S.add("act", lambda e, m=m: e.activation(out=m[:, 51:52], in_=m[:, 50:51], func=AF.Exp), reads=[RT, ("sm", b, 30)], writes=[RT, ("sm", b, 30)])
            dv(lambda e, m=m: e.tensor_scalar(out=m[:, 52:53], in0=m[:, 51:52], scalar1=1.0, scalar2=None, op0=ALU.add))
            dv(lambda e, m=m: e.reciprocal(out=m[:, 53:54], in_=m[:, 52:53]))
            dv(lambda e, m=m: e.tensor_tensor(out=m[:, 54:55], in0=m[:, 53:54], in1=m[:, 37:38], op=ALU.mult))
            dv(lambda e, m=m: e.tensor_tensor(out=m[:, 55:56], in0=m[:, 54:55], in1=m[:, 51:52], op=ALU.mult))
            dv(lambda e, q1=q1, tt=tt: e.tensor_copy(out=oh1[:, tt, :], in_=q1), wr_=[("oh", tt)])
            dv(lambda e, q2=q2, tt=tt: e.tensor_copy(out=oh2[:, tt, :], in_=q2), wr_=[("oh", tt)])
            dv(lambda e, m=m, tt=tt: e.tensor_copy(out=g12[:, tt, :], in_=m[:, 54:56]), wr_=[("oh", tt)])
        p1.close()
        ld("sync", lng[:], lnp[2], "lng", "lng")
        ld("sync", lnb[:], lnp[3], "lnb", "lnb")
        OH = [("oh", tt) for tt in range(NT)]
        NC_ = NT * NE
        ustr = S.sbuf("ustr", [128, 128], BF16)
        onesb = S.sbuf("onesb", [128, 128], BF16)
        eoff_s = S.sbuf("eoff_s", [128, NC_], F32)
        mskb = S.sbuf("mskb", [128, NC_], BF16)
        pos = S.sbuf("pos", [128, NT, NE], F32)
        base = S.sbuf("base", [128, NT, NE], F32)
        dst = S.sbuf("dst", [128, NT, NE], F32)
        dsel = S.sbuf("dsel", [128, NT, NE], F32)
        d12f = S.sbuf("d12f", [128, NT, 2], F32)
        d12i = S.sbuf("d12i", [128, NT, 2], I32)
        d12c = S.sbuf("d12c", [128, NT, 2], F32)
        d12ci = S.sbuf("d12ci", [128, NT, 2], I32)
        fl = lambda t: t[:].rearrange("p a b -> p (a b)")
        S.add("sync", lambda e: e.dma_start(out=eoff_s[:], in_=eoff), reads=OH, writes=["eoff"], dma=True, key="eoff")
        S.add("pool", lambda e: e.memset(onesb[:], 1.0), reads=OH, writes=["onesb"])
        S.add("pool", lambda e: e.memset(ident_f[:], 1.0), reads=["ident"], writes=["ident_f"])
        S.add("pool", lambda e: e.affine_select(out=ident_f[:], in_=ident_f[:], pattern=[[1, 128]], compare_op=ALU.is_gt, fill=0.0, base=0, channel_multiplier=-1),
              reads=["ident_f"], writes=["ident_f"])
        S.add("dve", lambda e: e.tensor_copy(out=ustr[:], in_=ident_f[:]), reads=["ident_f"] + OH, writes=["ustr"])
        S.add("dve", lambda e: e.tensor_tensor(out=fl(dst), in0=fl(oh1), in1=fl(oh2), op=ALU.add), reads=OH, writes=["dst"])
        S.add("dve", lambda e: e.tensor_copy(out=mskb[:], in_=fl(dst)), reads=["dst"], writes=["mskb"])
        S.add("pe", lambda e: e.matmul(Gps[0][:], lhsT=ustr[:], rhs=mskb[:], start=True, stop=True), reads=["ustr", "mskb"], writes=[("Gps", 0)])
        S.add("pe", lambda e: e.matmul(Gps[1][:], lhsT=onesb[:], rhs=mskb[:], start=True, stop=True), reads=["onesb", "mskb"], writes=[("Gps", 1)])
        S.add("act", lambda e: e.copy(out=fl(dst), in_=Gps[1][:]), reads=[("Gps", 1)], writes=["dst"])
        S.add("pool", lambda e: e.memset(base[:, 0, :], 0.0), reads=OH, writes=["base"])
        for t in range(1, NT):
            S.add("dve", lambda e, t=t: e.tensor_tensor(out=base[:, t, :], in0=base[:, t - 1, :], in1=dst[:, t - 1, :], op=ALU.add), reads=["base", "dst"], writes=["base"])
        S.add("dve", lambda e: e.tensor_tensor(out=fl(pos), in0=fl(base), in1=Gps[0][:], op=ALU.add), reads=["base", ("Gps", 0)], writes=["pos"])
        S.add("dve", lambda e: e.tensor_scalar(out=fl(dst), in0=fl(pos), scalar1=float(CAP), scalar2=1e6, op0=ALU.is_ge, op1=ALU.mult), reads=["pos"], writes=["dst"])
        S.add("dve", lambda e: e.tensor_tensor(out=fl(dst), in0=fl(dst), in1=fl(pos), op=ALU.add), reads=["pos", "dst"], writes=["dst"])
        S.add("dve", lambda e: e.tensor_tensor(out=fl(dst), in0=fl(dst), in1=eoff_s[:], op=ALU.add), reads=["dst", "eoff"], writes=["dst"])
        for k, oh in enumerate((oh1, oh2)):
            S.add("dve", lambda e, oh=oh: e.tensor_tensor(out=fl(dsel), in0=fl(dst), in1=fl(oh), op=ALU.mult), reads=["dst"] + OH, writes=["dsel"])
            S.add("dve", lambda e, k=k: e.reduce_sum(out=d12f[:, :, k], in_=dsel[:], axis=AX.X), reads=["dsel"], writes=["d12f"])
        S.add("dve", lambda e: e.tensor_copy(out=d12i[:], in_=d12f[:]), reads=["d12f"], writes=["d12i"])
        S.add("dve", lambda e: e.tensor_scalar(out=d12c[:], in0=d12f[:], scalar1=float(NE * CAP - 1), scalar2=None, op0=ALU.min), reads=["d12f"], writes=["d12c"])
        S.add("dve", lambda e: e.tensor_copy(out=d12ci[:], in_=d12c[:]), reads=["d12c"], writes=["d12ci"])
        S.add("dve", lambda e: e.tensor_scalar(out=d12c[:], in0=d12f[:], scalar1=float(NE * CAP), scalar2=None, op0=ALU.is_lt), reads=["d12f", "d12ci"], writes=["d12c"])
        S.add("dve", lambda e: e.tensor_tensor(out=g12[:], in0=g12[:], in1=d12c[:], op=ALU.mult), reads=["d12c"] + OH, writes=OH)
        if DEBUG:
            S.add("sync", lambda e: e.dma_start(out=dbg, in_=d12i[:].rearrange("p a b -> p (a b)")), reads=["d12i"], writes=["dbg"], dma=True, key="dbg")
            S.add("sync", lambda e: e.dma_start(out=dbg2, in_=g12[:].rearrange("p a b -> p (a b)")), reads=OH, writes=["dbg2"], dma=True, key="dbg2")
        XGZ = [("Xgz", i) for i in range(NE * CAP // 128)]
        for t in range(NT):
            for k in range(2):
                S.add("pool", lambda e, t=t, k=k: e.indirect_dma_start(
                    out=Xg, out_offset=bass.IndirectOffsetOnAxis(ap=d12i[:, t, k:k + 1], axis=0), in_=x1b[:, t, :], in_offset=None,
                    bounds_check=NE * CAP - 1, oob_is_err=False),
                    reads=["d12i", ("x1b", t)] + XGZ, writes=[("Xgs", t, k)], dma=True, key="xgs")
        XGS = [("Xgs", t, k) for t in range(NT) for k in range(2)]
        xg_s = [S.sbuf(f"xg_s{i}", [128, D], BF16) for i in range(2)]
        xgT = [S.sbuf(f"xgT{i}", [128, 8, 128], BF16) for i in range(2)]
        sg = [S.sbuf(f"sg{i}", [128, 256], F32) for i in range(2)]
        hb_ = [S.sbuf(f"hb{i}", [128, 256], BF16) for i in range(2)]
        hT = [S.sbuf(f"hT{i}", [128, 2, 128], BF16) for i in range(2)]
        ys = S.sbuf("ys", [128, D], F32)
        yg2 = S.sbuf("yg2", [128, D], F32)
        NBLK = CAP // 128
        stages = []
        cnt = 0
        for ex in range(NE):
            eb = ex % 2
            for bk in range(NBLK):
                b = cnt % 2
                cnt += 1
                r0 = ex * CAP + bk * 128

                def stT(ex=ex, eb=eb, bk=bk, b=b, r0=r0):
                    if bk == 0:
                        for c in range(8):
                            S.add("sync" if c < 4 else "act", lambda e, c=c: e.dma_start(out=wsf[:, c, :], in_=wgu[ex, :, c, :]), writes=[("wsfc", c)], dma=True, key=f"wsfc{c}")
                        for c in range(8):
                            if c % 2 == 0:
                                S.add("act", lambda e, c=c: e.copy(out=wgu_b[eb][:, c, :], in_=wsf[:, c, :]), reads=[("wsfc", c)], writes=[("wbigc", eb, c)])
                            else:
                                S.add("dve", lambda e, c=c: e.tensor_copy(out=wgu_b[eb][:, c, :], in_=wsf[:, c, :]), reads=[("wsfc", c)], writes=[("wbigc", eb, c)])
                    if bk == 1:
                        for c in range(2):
                            S.add("sync", lambda e, c=c: e.dma_start(out=wdf[:, c, :], in_=wd[ex, :, c, :]), writes=[("wdfc", c)], dma=True, key=f"wdfc{c}")
                        S.add("act", lambda e: e.copy(out=wd_b[eb][:, 0, :], in_=wdf[:, 0, :]), reads=[("wdfc", 0)], writes=[("wd_bc", eb, 0)])
                        S.add("dve", lambda e: e.tensor_copy(out=wd_b[eb][:, 1, :], in_=wdf[:, 1, :]), reads=[("wdfc", 1)], writes=[("wd_bc", eb, 1)])
                    S.add("sync", lambda e: e.dma_start(out=xg_s[b][:], in_=Xg[r0:r0 + 128, :]), reads=XGS, writes=[("xg_s", b)], dma=True, key=f"xg_s{b}")
                    for kc in range(8):
                        S.add("pe", lambda e, kc=kc: e.transpose(out=Tps[b][:, kc, :], in_=xg_s[b][:, kc * 128:(kc + 1) * 128], identity=ident[:]),
                              reads=[("xg_s", b), "ident"], writes=[("Tps", b)])
                    S.add("act", lambda e: e.copy(out=xgT[b][:], in_=Tps[b][:]), reads=[("Tps", b)], writes=[("xgT", b)])

                def stA(eb=eb, b=b):
                    for kc in range(8):
                        S.add("pe", lambda e, kc=kc: e.matmul(Gps[b][:], lhsT=xgT[b][:, kc, :], rhs=wgu_b[eb][:, kc, :], start=(kc == 0), stop=(kc == 7)),
                              reads=[("xgT", b), ("wbigc", eb, kc)], writes=[("Gps", b)])
                    S.add("act", lambda e: e.activation(out=sg[b][:], in_=Gps[b][:, 0:256], func=AF.Silu), reads=[("Gps", b)], writes=[("sg", b)])
                    S.add("dve", lambda e: e.tensor_tensor(out=hb_[b][:], in0=Gps[b][:, 256:512], in1=sg[b][:], op=ALU.mult),
                          reads=[("Gps", b), ("sg", b)], writes=[("hb", b)])

                def stB(b=b):
                    for dc in range(2):
                        S.add("pe", lambda e, dc=dc: e.transpose(out=T2ps[b][:, dc, :], in_=hb_[b][:, dc * 128:(dc + 1) * 128], identity=ident[:]),
                              reads=[("hb", b), "ident"], writes=[("T2ps", b)])
                    S.add("act", lambda e: e.copy(out=hT[b][:], in_=T2ps[b][:]), reads=[("T2ps", b)], writes=[("hT", b)])

                def stC(eb=eb, b=b, r0=r0, i=cnt - 1):
                    for half in range(2):
                        for dc in range(2):
                            S.add("pe", lambda e, half=half, dc=dc: e.matmul(Yps[0][:, half * 512:(half + 1) * 512], lhsT=hT[b][:, dc, :], rhs=wd_b[eb][:, dc, half * 512:(half + 1) * 512], start=(dc == 0), stop=(dc == 1)),
                                  reads=[("hT", b), ("wd_bc", eb, dc)], writes=[("Yps", 0)])
                    S.add("dve", lambda e: e.tensor_copy(out=ys[:, 0:512], in_=Yps[0][:, 0:512]), reads=[("Yps", 0)], writes=[("ys", 0)])
                    S.add("act", lambda e: e.copy(out=ys[:, 512:1024], in_=Yps[0][:, 512:1024]), reads=[("Yps", 0)], writes=[("ys", 1)])
                    S.add("sync", lambda e: e.dma_start(out=Yg[r0:r0 + 128, :], in_=ys[:]), reads=[("ys", 0), ("ys", 1)], writes=[("Yg", i)], dma=True, key="yso")

                stages.append((stT, stA, stB, stC))
        n_st = len(stages)
        for k in range(n_st + 3):
            for j in range(4):
                if 0 <= k - j < n_st:
                    stages[k - j][j]()
        YGA = [("Yg", i) for i in range(n_st)]
        for t in range(NT):
            for k, buf, res in ((0, junk, "junk"), (1, yg2, "yg2")):
                if t == 0:
                    S.add("pool", lambda e, buf=buf: e.memset(buf[:], 0.0), writes=[res])
                S.add("pool", lambda e, t=t, k=k, buf=buf: e.indirect_dma_start(
                    out=buf[:], out_offset=None, in_=Yg, in_offset=bass.IndirectOffsetOnAxis(ap=d12ci[:, t, k:k + 1], axis=0)),
                    reads=["d12ci", res] + YGA, writes=[res], dma=True, key=f"yg{k}")
                S.add("dve", lambda e, t=t, k=k, buf=buf: e.scalar_tensor_tensor(out=xs[:, t, :], in0=buf[:], scalar=g12[:, t, k:k + 1], in1=xs[:, t, :], op0=ALU.mult, op1=ALU.add),
                      reads=[res, ("xs", t)] + OH, writes=[("xs", t)])
        for tt in range(NT):
            layer_norm(tt, tt % 2, "lng", "lnb")
            S.add("pool", lambda e, tt=tt: e.dma_start(out=x2[tt * 128:(tt + 1) * 128, :], in_=xs[:, tt, :]), reads=[("xs", tt)], writes=[("x2", tt)], dma=True, key="x2o")
        S.emit()
    return nc


def k4_inputs(xcur, k3res, d, l):
    bc = lambda v: np.ascontiguousarray(np.broadcast_to(np.asarray(v, np.float32).reshape(1, -1), (128, v.size)))
    lnp = np.ascontiguousarray(np.stack([bc(d["ln1_g"][l]), bc(d["ln1_b"][l]), bc(d["ln2_g"][l]), bc(d["ln2_b"][l])], 0))
    wr = np.ascontiguousarray(np.concatenate([d["w_route_group"][l], d["w_route_expert"][l].transpose(1, 0, 2).reshape(D, 32)], axis=1))
    br = bc(np.concatenate([d["b_route_group"][l], d["b_route_expert"][l].reshape(32)]))
    wgu_h = np.ascontiguousarray(np.concatenate([d["w_expert_gate"][l], d["w_expert_up"][l]], axis=2).reshape(NE, 8, 128, 512).transpose(0, 2, 1, 3))
    wd_h = np.ascontiguousarray(d["w_expert_down"][l].reshape(NE, 2, 128, D).transpose(0, 2, 1, 3))
    perms = [_perm(_stream_dil(s)) for s in range(NSTREAM)]
    ocg = np.empty((128, 128, 512), k3res[0]["oc"].dtype)
    for c in range(NC):
        ocg[c::8] = k3res[c]["oc"].reshape(16, 128, 512)
    ocg = ocg.reshape(NC * TL, 512)
    maps = []
    for c in range(NC):
        O = k3res[c]["Oab"]
        nat = np.empty_like(O)
        for s in range(NSTREAM):
            nat[s][perms[s]] = O[s]
        maps.append({
            "x": np.ascontiguousarray(xcur[c * TL:(c + 1) * TL]),
            "Oa": np.ascontiguousarray(nat[:4].transpose(1, 0, 2).reshape(TL, 260)),
            "Ob": np.ascontiguousarray(nat[4:].reshape(3, 4, TL, 65).transpose(0, 2, 1, 3).reshape(3, TL, 260)),
            "oc": np.ascontiguousarray(ocg[c * TL:(c + 1) * TL]), "esk": bc(d["sinks"][l]), "wout": d["w_out"][l], "lnp": lnp, "wr": wr, "br": br,
            "wgu": wgu_h, "wd": wd_h,
            "eoff": np.ascontiguousarray(np.broadcast_to(np.tile(np.arange(NE, dtype=np.float32) * CAP, NT)[None, :], (128, NT * NE))),
        })
    return maps


_PROGS = {}


def _prog(name, fn):
    if name not in _PROGS:
        _PROGS[name] = fn()
    return _PROGS[name]


def kernel(**inputs):
    d = {k: np.asarray(v) for k, v in inputs.items()}
    x = np.ascontiguousarray(d["x"][0], dtype=np.float32)
    for l in range(4):
        r1 = _run(_prog("k1", build_k1), [{"xT": np.ascontiguousarray(x[c * TL:(c + 1) * TL].T), "w": d["w_in"][l]} for c in range(NC)])
        h = np.concatenate([r["h"] for r in r1], axis=0)
        r2 = _run(_prog("k2", build_k2), k2_inputs(h, d, l))
        r3 = _run(_prog("k3", build_k3), k3_inputs(h, r2, d))
        r4 = _run(_prog("k4", build_k4), k4_inputs(x, r3, d, l))
        x = np.concatenate([r["x2"] for r in r4], axis=0)
    return x[None].astype(np.float32)
```
